# Optimizing a Trainium2 kernel written in Bass

```python
import jax, jax.numpy as jnp
from jax import lax
import numpy as np

D_MODEL = 4096
BATCH = 4
SEQ = 4096
DEPTH = 1

D_A = D_MODEL // 2
HA_DK = 128
H_A = D_A // HA_DK
HA_DV = D_A // H_A
D_B = D_MODEL // 2
H_B = 8
DV_B = D_B // H_B
DK_B = DV_B // 2
D_QK_B = H_B * DK_B
CONV_W = 4
CHUNK = 64
EPS = 1e-6

COLS = (D_A, D_A, D_A, D_A, D_A,
        D_QK_B, D_QK_B, D_B, D_B, D_B, H_B, H_B,
        D_MODEL, D_MODEL)
D_IN = sum(COLS)
SPLIT_IDX = tuple(int(v) for v in np.cumsum(COLS)[:-1])

kernel_name = "hgrn2_mlstm_gated_parallel_hybrid"


def rmsnorm(x, g):
    xf = x.astype(jnp.float32)
    r = lax.rsqrt(jnp.mean(xf * xf, axis=-1, keepdims=True) + EPS)
    return (xf * r).astype(x.dtype) * g


def head_rmsnorm(o, g, n_heads):
    B, S, W = o.shape
    of = o.astype(jnp.float32).reshape(B, S, n_heads, W // n_heads)
    of = of * lax.rsqrt(jnp.mean(of * of, axis=-1, keepdims=True) + EPS)
    return of.reshape(B, S, W).astype(o.dtype) * g


def to_chunks(a, n_heads, d):
    B, S, _ = a.shape
    return a.reshape(B, S // CHUNK, CHUNK, n_heads, d).transpose(0, 3, 1, 2, 4)


def from_chunks(a):
    B, H, N, C, d = a.shape
    return a.transpose(0, 2, 3, 1, 4).reshape(B, N * C, H * d)


def causal_conv(u, w, b):
    S = u.shape[1]
    up = jnp.pad(u, ((0, 0), (CONV_W - 1, 0), (0, 0)))
    y = b
    for k in range(CONV_W):
        y = y + w[k] * up[:, k:k + S]
    return y


MASK = np.tril(np.ones((CHUNK, CHUNK), dtype=bool))


def hgrn2_mixer(q, f_pre, i, lb):
    dt = q.dtype
    q, f_pre, i, lb = (a.astype(jnp.float32) for a in (q, f_pre, i, lb))
    f = lb + (1.0 - lb) * jax.nn.sigmoid(f_pre)
    logf = jnp.log(f)
    k = 1.0 - f
    qc, kc, vc, lfc = (to_chunks(a, H_A, HA_DK if a is not i else HA_DV) for a in (q, k, i, logf))
    b = jnp.cumsum(lfc, axis=-2)
    b_mid = b[..., CHUNK // 2 - 1:CHUNK // 2, :]
    q_rel = qc * jnp.exp(b - b_mid)
    k_rel = kc * jnp.exp(b_mid - b)
    scores = jnp.einsum('bhncd,bhnsd->bhncs', q_rel, k_rel)
    scores = jnp.where(MASK, scores, 0.0)
    o_intra = jnp.einsum('bhncs,bhnsv->bhncv', scores, vc)
    g = b[..., -1, :]
    q_in = qc * jnp.exp(b)
    k_out = kc * jnp.exp(g[..., None, :] - b)
    U = jnp.einsum('bhncd,bhncv->bhndv', k_out, vc)

    def step(S, inp):
        q_in_j, g_j, U_j = inp
        o = jnp.einsum('bhcd,bhdv->bhcv', q_in_j, S)
        S = jnp.exp(g_j)[..., None] * S + U_j
        return S, o

    B = q.shape[0]
    S0 = jnp.zeros((B, H_A, HA_DK, HA_DV), jnp.float32)
    xs = (jnp.moveaxis(q_in, 2, 0), jnp.moveaxis(g, 2, 0), jnp.moveaxis(U, 2, 0))
    _, o_inter = lax.scan(step, S0, xs)
    o = o_intra + jnp.moveaxis(o_inter, 0, 2)
    return from_chunks(o).astype(dt)


def mlstm_mixer(q, k, v, i_pre, f_pre):
    dt = v.dtype
    q, k, v, i_pre, f_pre = (a.astype(jnp.float32) for a in (q, k, v, i_pre, f_pre))
    B, S, _ = v.shape
    N = S // CHUNK
    qc = to_chunks(q, H_B, DK_B)
    kc = to_chunks(k, H_B, DK_B) * (DK_B ** -0.5)
    vc = to_chunks(v, H_B, DV_B)
    logf = jax.nn.log_sigmoid(f_pre).reshape(B, N, CHUNK, H_B).transpose(0, 3, 1, 2)
    ig = i_pre.reshape(B, N, CHUNK, H_B).transpose(0, 3, 1, 2)
    b = jnp.cumsum(logf, axis=-1)

    def step(carry, inp):
        Cs, ns, m = carry
        q_j, k_j, v_j, b_j, ig_j = inp
        g_j = b_j[..., -1]
        logD = b_j[..., :, None] - b_j[..., None, :] + ig_j[..., None, :]
        logD = jnp.where(MASK, logD, -jnp.inf)
        m_t = jnp.maximum(b_j + m[..., None], jnp.max(logD, axis=-1))
        inter = jnp.exp(b_j + m[..., None] - m_t)
        Dw = jnp.exp(logD - m_t[..., None]) * jnp.einsum('bhcd,bhsd->bhcs', q_j, k_j)
        num = inter[..., None] * jnp.einsum('bhcd,bhdv->bhcv', q_j, Cs) \
            + jnp.einsum('bhcs,bhsv->bhcv', Dw, v_j)
        den = inter * jnp.einsum('bhcd,bhd->bhc', q_j, ns) + jnp.sum(Dw, axis=-1)
        h = num / jnp.maximum(jnp.abs(den), jnp.exp(-m_t))[..., None]
        log_w = g_j[..., None] - b_j + ig_j
        m_new = jnp.maximum(g_j + m, jnp.max(log_w, axis=-1))
        decay = jnp.exp(g_j + m - m_new)
        w = jnp.exp(log_w - m_new[..., None])
        Cs = decay[..., None, None] * Cs + jnp.einsum('bhc,bhcd,bhcv->bhdv', w, k_j, v_j)
        ns = decay[..., None] * ns + jnp.einsum('bhc,bhcd->bhd', w, k_j)
        return (Cs, ns, m_new), h

    carry0 = (jnp.zeros((B, H_B, DK_B, DV_B), jnp.float32),
              jnp.zeros((B, H_B, DK_B), jnp.float32),
              jnp.zeros((B, H_B), jnp.float32))
    xs = tuple(jnp.moveaxis(a, 2, 0) for a in (qc, kc, vc, b, ig))
    _, h = lax.scan(step, carry0, xs)
    return from_chunks(jnp.moveaxis(h, 0, 2)).astype(dt)


def setup_inputs(seed: int = 0) -> dict:
    key = jax.random.key(seed)
    ks = jax.random.split(key, 16)
    f32 = jnp.float32
    nrm = lambda k, shape, s: jax.random.normal(k, shape, f32) * s
    x = nrm(ks[0], (BATCH, SEQ, D_MODEL), 1.0)
    g_pre = 1.0 + nrm(ks[1], (DEPTH, D_MODEL), 0.02)
    w_in = nrm(ks[2], (DEPTH, D_MODEL, D_IN), D_MODEL ** -0.5)
    lb_logits = nrm(ks[3], (DEPTH + 1, D_A), 0.5)
    conv_w = nrm(ks[4], (DEPTH, CONV_W, 2 * D_QK_B), CONV_W ** -0.5)
    conv_b = nrm(ks[5], (DEPTH, 2 * D_QK_B), 0.02)
    b_ig = nrm(ks[6], (DEPTH, H_B), 0.1)
    b_fg = jnp.linspace(3.0, 6.0, H_B, dtype=f32)[None, :] + nrm(ks[7], (DEPTH, H_B), 0.1)
    g_norm_a = 1.0 + nrm(ks[8], (DEPTH, D_A), 0.02)
    g_norm_b = 1.0 + nrm(ks[9], (DEPTH, D_B), 0.02)
    w_up_a = nrm(ks[10], (DEPTH, D_A, D_MODEL), D_A ** -0.5)
    w_up_b = nrm(ks[11], (DEPTH, D_B, D_MODEL), D_B ** -0.5)
    w_out = nrm(ks[12], (DEPTH, D_MODEL, D_MODEL), D_MODEL ** -0.5)
    g_post = 1.0 + nrm(ks[13], (DEPTH, D_MODEL), 0.02)
    return {"x": x, "g_pre": g_pre, "w_in": w_in, "lb_logits": lb_logits,
            "conv_w": conv_w, "conv_b": conv_b, "b_ig": b_ig, "b_fg": b_fg,
            "g_norm_a": g_norm_a, "g_norm_b": g_norm_b, "w_up_a": w_up_a,
            "w_up_b": w_up_b, "w_out": w_out, "g_post": g_post}


def reference(x, g_pre, w_in, lb_logits, conv_w, conv_b, b_ig, b_fg,
              g_norm_a, g_norm_b, w_up_a, w_up_b, w_out, g_post):
    lb_all = jnp.cumsum(jax.nn.softmax(lb_logits.astype(jnp.float32), axis=0), axis=0)
    for l in range(DEPTH):
        h = rmsnorm(x, g_pre[l])
        p = jnp.einsum('bsd,de->bse', h, w_in[l])
        (qa, fa, ia, oga, za, qb, kb, vb, ogb, zb, igb, fgb, gate_a, gate_b) = \
            jnp.split(p, SPLIT_IDX, axis=-1)
        oa = hgrn2_mixer(qa, fa, ia, lb_all[l].astype(x.dtype))
        oa = head_rmsnorm(oa, g_norm_a[l], H_A) * jax.nn.sigmoid(oga) * jax.nn.silu(za)
        qk = jax.nn.silu(causal_conv(jnp.concatenate([qb, kb], axis=-1), conv_w[l], conv_b[l]))
        qb_c, kb_c = qk[..., :D_QK_B], qk[..., D_QK_B:]
        ob = mlstm_mixer(qb_c, kb_c, vb, igb + b_ig[l], fgb + b_fg[l])
        ob = head_rmsnorm(ob, g_norm_b[l], H_B) * jax.nn.sigmoid(ogb) * jax.nn.silu(zb)
        ya = jnp.einsum('bse,ed->bsd', oa, w_up_a[l])
        yb = jnp.einsum('bse,ed->bsd', ob, w_up_b[l])
        y = jax.nn.sigmoid(gate_a) * ya + jax.nn.sigmoid(gate_b) * yb
        out = jnp.einsum('bsd,de->bse', y, w_out[l])
        x = x + rmsnorm(out, g_post[l])
    return x
```

```python
import numpy as np
from collections import deque
from contextlib import ExitStack

import concourse.bass as bass
import concourse.mybir as mybir
from concourse.bass_utils import run_bass_kernel_spmd

F32 = mybir.dt.float32
BF16 = mybir.dt.bfloat16
AF = mybir.ActivationFunctionType
ALU = mybir.AluOpType

P = 128
D = 4096
KC = 32
T = 1024
TB = 512
NTB = T // TB
NTOK = 2048
EPS = 1e-6

OFF = dict(qa=0, fa=2048, ia=4096, oga=6144, za=8192, qb=10240, kb=11264, vb=12288,
           ogb=14336, zb=16384, igb=18432, fgb=18440, ga=18448, gb=22544)


def _tile_list():
    tl = []
    for h in range(16):
        for nm in ("Af", "Aq", "Ai", "Aog", "Az"):
            tl.append((nm, h))
    for h in range(8):
        for nm in ("Bfg", "Big", "Bk", "Bq", "Bv0", "Bv1", "Bog0", "Bz0", "Bog1", "Bz1"):
            tl.append((nm, h))
    for c in range(32):
        tl.append(("Ga", c))
        tl.append(("Gb", c))
    for c in range(32):
        tl.append(("Up", c))
    return tl


TILES = _tile_list()
TIDX = {t: i for i, t in enumerate(TILES)}
NT = len(TILES)


def _tile_cols(nm, h):
    ar = np.arange(128)
    if nm == "Af":
        return OFF["fa"] + h * 128 + ar
    if nm == "Aq":
        return OFF["qa"] + h * 128 + ar
    if nm == "Ai":
        return OFF["ia"] + h * 128 + ar
    if nm == "Aog":
        return OFF["oga"] + h * 128 + ar
    if nm == "Az":
        return OFF["za"] + h * 128 + ar
    if nm == "Bfg":
        return np.full(128, OFF["fgb"] + h)
    if nm == "Big":
        return np.full(128, OFF["igb"] + h)
    if nm == "Bk":
        return OFF["kb"] + h * 128 + ar
    if nm == "Bq":
        return OFF["qb"] + h * 128 + ar
    if nm in ("Bv0", "Bv1"):
        return OFF["vb"] + h * 256 + (128 if nm == "Bv1" else 0) + ar
    if nm in ("Bog0", "Bog1"):
        return OFF["ogb"] + h * 256 + (128 if nm == "Bog1" else 0) + ar
    if nm in ("Bz0", "Bz1"):
        return OFF["zb"] + h * 256 + (128 if nm == "Bz1" else 0) + ar
    if nm == "Ga":
        return OFF["ga"] + h * 128 + ar
    if nm == "Gb":
        return OFF["gb"] + h * 128 + ar
    raise KeyError(nm)


class Sem:
    def __init__(self, h, step):
        self.h = h
        self.step = step
        self.n = 0


class Eng:
    def __init__(self, name, sem):
        self.name = name
        self.sem = sem
        self.q = []
        self.waited = {}


class Buf:
    __slots__ = ("name", "w", "r")

    def __init__(self, name):
        self.name = name
        self.w = None
        self.r = {}


class Prog:
    def __init__(self, nc, es):
        self.nc = nc
        self.es = es
        self.sems = []
        self.bufs = []
        mk = lambda nm, st: self.new_sem(nm, st)
        self.PE = Eng("tensor", mk("s_pe", 1))
        self.ACT = Eng("scalar", mk("s_act", 1))
        self.DVE = Eng("vector", mk("s_dve", 1))
        self.POOL = Eng("gpsimd", mk("s_pool", 1))
        self.SP = Eng("sync", mk("s_sp", 1))
        self.engs = [self.PE, self.ACT, self.DVE, self.POOL, self.SP]

    def new_sem(self, name, step):
        s = Sem(self.es.enter_context(self.nc.semaphore(name)), step)
        self.sems.append(s)
        return s

    def buf(self, name):
        b = Buf(name)
        self.bufs.append(b)
        return b

    def _wait(self, eng, sem, val, raw=False):
        if sem is eng.sem and not raw:
            return
        if eng.waited.get(sem, 0) >= val:
            return
        eng.waited[sem] = val
        eng.q.append(lambda h, s=sem.h, v=val: h.wait_ge(s, v))

    def emit(self, eng, fn, reads=(), writes=(), sem=None):
        for b in reads:
            if b.w is not None:
                self._wait(eng, b.w[0], b.w[1], raw=True)
        for b in writes:
            if b.w is not None:
                self._wait(eng, b.w[0], b.w[1])
            for s, v in b.r.items():
                self._wait(eng, s, v)
        s = sem if sem is not None else eng.sem
        s.n += 1
        val = s.n * s.step
        eng.q.append(lambda h, f=fn, sh=s.h, st=s.step: f(h).then_inc(sh, st))
        tok = (s, val)
        for b in reads:
            if b.r.get(s, 0) < val:
                b.r[s] = val
        for b in writes:
            b.w = tok
            b.r = {}
        return tok

    def barrier(self):
        for e in self.engs:
            for s in self.sems:
                if s.n > 0:
                    self._wait(e, s, s.n * s.step)
        for b in self.bufs:
            b.w = None
            b.r = {}

    def final_wait(self, eng):
        for s in self.sems:
            if s.n > 0:
                self._wait(eng, s, s.n * s.step)


class Tl:
    __slots__ = ("ap", "b")

    def __init__(self, ap, b):
        self.ap = ap
        self.b = b


def build_program(stop=None, skip_pre=False, nt=NT, nwo=8):
    nc = bass.Bass("TRN2", target_bir_lowering=False)
    xm = nc.dram_tensor("xm", [NTOK, D], F32, kind="ExternalInput").ap()
    xp = nc.dram_tensor("xp", [NTOK, D], F32, kind="ExternalInput").ap()
    wt = nc.dram_tensor("wt", [nt, P, KC * 128], F32, kind="ExternalInput").ap()
    wo = nc.dram_tensor("wo", [nwo, P, KC * 512], F32, kind="ExternalInput").ap()
    prm_d = nc.dram_tensor("prm", [P, 168], F32, kind="ExternalInput").ap()
    cst_d = nc.dram_tensor("cst", [P, 128 + 512 + 512], F32, kind="ExternalInput").ap()
    gpre_d = nc.dram_tensor("gpre", [1, D], F32, kind="ExternalInput").ap()
    gpost_d = nc.dram_tensor("gpost", [1, D], F32, kind="ExternalInput").ap()
    y_d = nc.dram_tensor("y", [NTOK, D], F32, kind="ExternalOutput").ap()
    dbg_d = nc.dram_tensor("dbg", [2, P, 32768], BF16, kind="ExternalOutput").ap() if stop else None
    ysc = nc.dram_tensor("ysc", [32, P, T], BF16).ap()
    osc = nc.dram_tensor("osc", [T, D], F32).ap()

    with ExitStack() as es:
        pg = Prog(nc, es)
        PE, ACT, DVE, POOL, SP = pg.PE, pg.ACT, pg.DVE, pg.POOL, pg.SP
        sb = lambda nm, shape, dt: es.enter_context(nc.sbuf_tensor(nm, shape, dt))
        R1 = sb("R1", [P, 32768], BF16)
        R2 = sb("R2", [P, 32768], BF16)
        R3 = sb("R3", [P, 29696], BF16)
        PA = sb("PA", [P, 16, 128], F32)
        PB = sb("PB", [P, 8, 257], F32)
        sm = sb("sm", [P, 256], F32)
        prm = sb("prm_s", [P, 168], F32)
        cF = sb("cF", [P, 128 + 512], F32)
        cB = sb("cB", [P, 128 + 512], BF16)
        onesb = sb("onesb", [P, 128], BF16)
        banks = [es.enter_context(nc.psum_tensor("pb%d" % i, [P, 512], F32)) for i in range(8)]
        bk = [Tl(b[:, :], pg.buf("pb%d" % i)) for i, b in enumerate(banks)]
        IP0, IP1, TP, SC, OA0, OA1, UX, MX = bk

        l0 = prm[:, 0:16]
        l1 = prm[:, 16:32]
        gna = prm[:, 32:48]
        gnb = prm[:, 48:64]
        cw = prm[:, 64:128].rearrange("p (h k) -> p h k", k=4)
        cbias = prm[:, 128:144]
        big = prm[:, 144:152]
        bfg = prm[:, 152:160]
        flag = prm[:, 160:161]
        lb = sm[:, 0:16]
        oml = sm[:, 16:32]
        noml = sm[:, 32:48]
        pexpA = sm[:, 48:64]
        pexpB = sm[:, 64:72]
        histq = sm[:, 72:96].rearrange("p (h k) -> p h k", k=3)
        histk = sm[:, 96:120].rearrange("p (h k) -> p h k", k=3)
        ssq = sm[:, 120:128]
        rr = sm[:, 128:136]
        ssqp = sm[:, 136:200].rearrange("p (t c) -> p t c", c=8)
        nbfg = sm[:, 200:208]
        identF = cF[:, 0:128]
        rmask = cF[:, 128:640]
        identB = cB[:, 0:128]
        maskBD = cB[:, 128:640]

        hT = R1[:, :].rearrange("p (k t) -> p k t", k=KC)
        oT = R2[:, :].rearrange("p (k t) -> p k t", k=KC)
        woslot = [Tl(oT[:, :, s * 512:(s + 1) * 512], pg.buf("wo%d" % s)) for s in range(2)]

        dsem = lambda nm: pg.new_sem(nm, 16)
        s_c = dsem("d_c")
        s_x = [dsem("d_x0"), dsem("d_x1")]
        s_w = [dsem("d_w0"), dsem("d_w1")]
        s_wo = [dsem("d_wo0"), dsem("d_wo1")]
        s_gb = dsem("d_gb")
        s_ys = [dsem("d_ys0"), dsem("d_ys1")]
        s_yl = dsem("d_yl")
        s_ot = [dsem("d_ot%d" % i) for i in range(4)]
        s_ob = [dsem("d_ob0"), dsem("d_ob1")]
        s_xb = [dsem("d_xb0"), dsem("d_xb1")]
        s_st = [dsem("d_st0"), dsem("d_st1")]

        def act(out, in_, func, reads, writes, scale=1.0, bias=0.0, accum=None):
            def f(h):
                if accum is None:
                    return h.activation(out=out, in_=in_, func=func, bias=bias, scale=scale)
                return h.activation(out=out, in_=in_, func=func, bias=bias, scale=scale,
                                    accum_out=accum)
            return pg.emit(ACT, f, reads, writes)

        def tt(out, in0, in1, op, reads, writes, eng=None):
            return pg.emit(eng or DVE, lambda h: h.tensor_tensor(out=out, in0=in0, in1=in1, op=op),
                           reads, writes)

        def ts(out, in0, s1, op0, reads, writes, s2=None, op1=None, eng=None):
            def f(h):
                if op1 is None:
                    return h.tensor_scalar(out=out, in0=in0, scalar1=s1, scalar2=None, op0=op0)
                return h.tensor_scalar(out=out, in0=in0, scalar1=s1, scalar2=s2, op0=op0, op1=op1)
            return pg.emit(eng or DVE, f, reads, writes)

        def stt(out, in0, scalar, in1, op0, op1, reads, writes):
            return pg.emit(DVE, lambda h: h.scalar_tensor_tensor(out=out, in0=in0, scalar=scalar,
                                                                  in1=in1, op0=op0, op1=op1),
                           reads, writes)

        def act_sigmoid(out, in_, tmp, reads, writes, scale=1.0, bias=0.0):
            act(tmp.ap if isinstance(tmp, Tl) else tmp, in_, AF.Exp, reads, [tmp.b], scale=-scale, bias=bias)
            act(tmp.ap, tmp.ap, AF.Ln, [tmp.b], [tmp.b], bias=1.0)
            return act(out, tmp.ap, AF.Exp, [tmp.b], writes, scale=-1.0)

        def act_rpow(out, in_, reads, writes, scale, bias, p):
            act(out, in_, AF.Ln, reads, writes, scale=scale, bias=bias)
            return act(out, out, AF.Exp, writes, writes, scale=-p)

        def cpy(out, in_, reads, writes, eng=None):
            return pg.emit(eng or DVE, lambda h: h.tensor_copy(out=out, in_=in_), reads, writes)

        def recip(out, in_, reads, writes):
            return pg.emit(DVE, lambda h: h.reciprocal(out=out, in_=in_), reads, writes)

        def dma(eng, sem, out, in_, reads, writes):
            return pg.emit(eng, lambda h: h.dma_start(out=out, in_=in_), reads, writes, sem=sem)

        def mm_group(specs, reads, writes):
            def f(h):
                ins = None
                for (o, l, r, st, sp) in specs:
                    ins = h.matmul(o, l, r, start=st, stop=sp)
                return ins
            return pg.emit(PE, f, reads, writes)

        def tr_group(specs, reads, writes):
            def f(h):
                ins = None
                for (o, i, idn) in specs:
                    ins = h.transpose(o, i, idn)
                return ins
            return pg.emit(PE, f, reads, writes)

        b_prm = pg.buf("prm")
        b_c = pg.buf("consts")
        b_sm = pg.buf("sm")
        b_PA = [pg.buf("PA%d" % h) for h in range(16)]
        b_PB = [pg.buf("PB%d" % h) for h in range(8)]
        dma(SP, s_c, prm[:, :], prm_d[:, :], [], [b_prm])
        dma(SP, s_c, cF[:, :], cst_d[:, 0:640], [], [b_c])
        dma(POOL, s_c, cB[:, 0:128], cst_d[:, 0:128], [], [b_c])
        dma(POOL, s_c, cB[:, 128:640], cst_d[:, 640:1152], [], [b_c])
        pg.emit(DVE, lambda h: h.memset(onesb[:, :], 1.0), [], [b_c])
        pg.emit(DVE, lambda h: h.memset(sm[:, :], 0.0), [], [b_sm])
        pg.emit(DVE, lambda h: h.memset(PA[:, :, :], 0.0), [], b_PA)
        pg.emit(DVE, lambda h: h.memset(PB[:, :, :], 0.0), [], b_PB)
        tt(lb, l0, l1, ALU.subtract, [b_prm], [b_sm])
        act(lb, lb, AF.Exp, [b_sm], [b_sm], scale=-1.0)
        act(lb, lb, AF.Ln, [b_sm], [b_sm], bias=1.0)
        act(lb, lb, AF.Exp, [b_sm], [b_sm], scale=-1.0)
        ts(nbfg, bfg, -1.0, ALU.mult, [b_prm], [b_sm])
        ts(oml, lb, -1.0, ALU.mult, [b_sm], [b_sm], s2=1.0, op1=ALU.add)
        ts(noml, lb, -1.0, ALU.add, [b_sm], [b_sm])
        pg.barrier()

        wslot = [Tl(R3[:, i * 4096:(i + 1) * 4096].rearrange("p (k j) -> p k j", k=KC),
                    pg.buf("ws%d" % i)) for i in range(2)]
        wctr = [0]

        def wload(name, idx):
            s = wctr[0] % 2
            wctr[0] += 1
            t = wslot[s]
            dma(POOL, s_w[s], t.ap, wt[TIDX[(name, idx)]].rearrange("p (k j) -> p k j", k=KC),
                [], [t.b])
            return t

        ipc = [0]

        def ipbank():
            b = (IP0, IP1)[ipc[0] % 2]
            ipc[0] += 1
            return b

        def inproj(w, tb, bank, k0=0, k1=KC, src=None):
            src = hT if src is None else src
            specs = [(bank.ap, w.ap[:, k, :], src[:, k, tb * TB:(tb + 1) * TB], k == k0, k == k1 - 1)
                     for k in range(k0, k1)]
            return mm_group(specs, [w.b], [bank.b])

        class Work:
            def __init__(self):
                self.off = 8192

            def reset(self):
                self.off = 8192

            def bf(self, name, n):
                a = R3[:, self.off:self.off + n]
                self.off += n + (n % 2)
                assert self.off <= 29696, "work overflow"
                return Tl(a, pg.buf(name))

            def f32(self, name, n):
                a = R3[:, self.off:self.off + 2 * n].bitcast(F32)
                self.off += 2 * n
                assert self.off <= 29696, "work overflow"
                return Tl(a, pg.buf(name))

        wk = Work()

        def phase_a0(xsrc, row0, gb_src):
            xt = [Tl(R3[:, i * 8192:(i + 1) * 8192].bitcast(F32), pg.buf("xt%d" % i)) for i in range(2)]
            xs = Tl(R3[:, 16384:20480], pg.buf("xs"))
            gbt = Tl(R3[:, 20480:28672].bitcast(F32), pg.buf("gbt"))
            b_ss = pg.buf("ssq")
            dma(SP, s_gb, gbt.ap, gb_src[0:1, :].partition_broadcast(P), [], [gbt.b])
            tpb = [IP0, IP1, TP, SC]
            for t8 in range(T // P):
                x = xt[t8 % 2]
                dma(SP, s_x[t8 % 2], x.ap, xsrc[row0 + t8 * P: row0 + (t8 + 1) * P, :], [], [x.b])
                act(xs.ap, x.ap, AF.Square, [x.b], [xs.b, b_ss], accum=ssq[:, t8:t8 + 1])
                act_rpow(rr[:, t8:t8 + 1], ssq[:, t8:t8 + 1], [b_ss], [b_ss], 1.0 / D, EPS, 0.5)
                stt(xs.ap, x.ap, rr[:, t8:t8 + 1], gbt.ap, ALU.mult, ALU.mult, [x.b, gbt.b, b_ss], [xs.b])
                for g in range(4):
                    bank = tpb[g]
                    bv = bank.ap.bitcast(BF16)
                    specs = [(bv[:, i * 128:(i + 1) * 128], xs.ap[:, (g * 8 + i) * 128:(g * 8 + i + 1) * 128], identB)
                             for i in range(8)]
                    tr_group(specs, [xs.b], [bank.b])
                    dst = hT[:, g * 8:(g + 1) * 8, t8 * P:(t8 + 1) * P]
                    srcv = bv.rearrange("p (k t) -> p k t", k=8)
                    if g % 2 == 0:
                        act(dst, srcv, AF.Copy, [bank.b], [])
                    else:
                        cpy(dst, srcv, [bank.b], [])
            pg.barrier()

        pend = deque()
        late = []

        def defer(fn):
            pend.append(fn)

        def drain(k):
            while k > 0 and pend:
                pend.popleft()()
                k -= 1

        def flush():
            while pend:
                pend.popleft()()

        def run_late():
            while late:
                late.pop(0)()

        def group(w, tb, evac, ndrain):
            drain(ndrain)
            bank = ipbank()
            inproj(w, tb, bank)
            run_late()
            evac(bank)

        b_pexA = pg.buf("pexA")
        b_pexB = pg.buf("pexB")
        b_hist = pg.buf("hist")

        def heads_a(main):
            wk.reset()
            sig = wk.f32("sig", 512)
            lf = wk.f32("lf", 512)
            bb = wk.f32("bb", 512)
            e1 = [wk.f32("e1_%d" % i, 512) for i in range(2)]
            eg = [[wk.f32("eg%d%d" % (p_, i), 8) for i in range(2)] for p_ in range(2)]
            kt = [[wk.bf("kt%d%d" % (p_, i), 512) for i in range(2)] for p_ in range(2)]
            it = [wk.bf("it%d" % i, 512) for i in range(2)]
            vk = [wk.bf("vk%d" % i, 1024) for i in range(2)]
            if main:
                qt = [wk.bf("qt%d" % i, 512) for i in range(2)]
                gate = [wk.bf("gate%d" % i, 512) for i in range(2)]
                sz = wk.bf("sz", 512)
                scm = [wk.bf("scm%d" % i, 512) for i in range(2)]
                sball = [wk.bf("sball%d" % i, 1024) for i in range(2)]
                sq = [wk.bf("sq%d" % i, 512) for i in range(2)]
                rs = wk.f32("rs", 512)
                tmp = wk.f32("tmp", 512)
                oab = [OA0, OA1]
            ubanks = [UX, TP, MX] if main else [UX, SC, MX, OA0]
            uc = [0]
            nd = 3
            nd_f = 1 if main else 4
            for h in range(16):
                par = h % 2
                hc = slice(h, h + 1)
                w = wload("Af", h)
                for tb in range(NTB):
                    def ev(bank, tb=tb):
                        act(sig.ap, bank.ap, AF.Exp, [bank.b], [sig.b], scale=-1.0)
                        act(lf.ap, sig.ap, AF.Ln, [sig.b], [lf.b], bias=1.0)
                        act(sig.ap, lf.ap, AF.Exp, [lf.b], [sig.b], scale=-1.0)
                        act(lf.ap, sig.ap, AF.Ln, [sig.b], [lf.b], scale=oml[:, hc], bias=lb[:, hc])
                        pg.emit(DVE, lambda hh, o=bb.ap, d1=lf.ap: hh.tensor_tensor_scan(
                            out=o, data0=rmask, data1=d1, initial=0.0, op0=ALU.mult, op1=ALU.add),
                            [lf.b], [bb.b])
                        act(e1[tb].ap, bb.ap, AF.Exp, [bb.b], [e1[tb].b])
                        act(lf.ap, bb.ap, AF.Exp, [bb.b], [lf.b], scale=-1.0)
                        stt(kt[par][tb].ap, sig.ap, -1.0, lf.ap, ALU.add, ALU.mult, [sig.b, lf.b], [kt[par][tb].b])
                        cpy(eg[par][tb].ap, e1[tb].ap[:, 63:512:64], [e1[tb].b], [eg[par][tb].b])
                    group(w, tb, ev, nd_f)
                w = wload("Ai", h)
                for tb in range(NTB):
                    def ev(bank, tb=tb):
                        act(it[tb].ap, bank.ap, AF.Copy, [bank.b], [it[tb].b])
                    group(w, tb, ev, nd_f)
                flush()
                for tb in range(NTB):
                    tpv = TP.ap.bitcast(BF16)
                    specs = [(tpv[:, b4 * 128:(b4 + 1) * 128], it[tb].ap[:, b4 * 128:(b4 + 1) * 128], identB)
                             for b4 in range(4)]
                    specs += [(tpv[:, 512 + b4 * 128:512 + (b4 + 1) * 128],
                               kt[par][tb].ap[:, b4 * 128:(b4 + 1) * 128], identB) for b4 in range(4)]
                    tr_group(specs, [it[tb].b, kt[par][tb].b], [TP.b])
                    act(vk[tb].ap, tpv, AF.Copy, [TP.b], [vk[tb].b])
                for tb in range(NTB):
                    vtok = vk[tb].ap[:, 0:512].rearrange("p (b v) -> p b v", b=4)
                    ktok = vk[tb].ap[:, 512:1024].rearrange("p (b v) -> p b v", b=4)
                    if main:
                        def sb0(tb=tb, h=h, hc=hc):
                            act(sball[tb].ap[:, 0:128], PA[:, h, :], AF.Copy, [b_PA[h], b_pexA], [sball[tb].b],
                                scale=pexpA[:, hc])
                        defer(sb0)
                    for c in range(8):
                        def step(c=c, tb=tb, h=h, hc=hc, par=par, vtok=vtok, ktok=ktok):
                            b4 = c // 2
                            rows = slice((c % 2) * 64, (c % 2) * 64 + 64)
                            ub = ubanks[uc[0] % len(ubanks)]
                            uc[0] += 1
                            mm_group([(ub.ap[:, 0:128], ktok[rows, b4, :], vtok[rows, b4, :], True, True)],
                                     [vk[tb].b], [ub.b])
                            egt = eg[par][tb]
                            pe_ap = pexpA[:, hc] if c == 0 else egt.ap[:, c - 1:c]
                            stt(PA[:, h, :], PA[:, h, :], pe_ap, ub.ap[:, 0:128], ALU.mult, ALU.add,
                                [ub.b, egt.b, b_pexA], [b_PA[h]])
                            if c < 7:
                                if main:
                                    act(sball[tb].ap[:, (c + 1) * 128:(c + 2) * 128], PA[:, h, :], AF.Copy,
                                        [b_PA[h], egt.b], [sball[tb].b], scale=egt.ap[:, c:c + 1])
                            else:
                                cpy(pexpA[:, hc], egt.ap[:, 7:8], [egt.b], [b_pexA])
                        defer(step)
                if main:
                    w = wload("Aq", h)
                    for tb in range(NTB):
                        def ev(bank, tb=tb, par=par):
                            stt(qt[tb].ap, bank.ap, noml[:, hc], e1[tb].ap, ALU.mult, ALU.mult,
                                [bank.b, e1[tb].b], [qt[tb].b])

                            def scores(tb=tb, par=par):
                                specs = [(SC.ap[:, b4 * 128:(b4 + 1) * 128], kt[par][tb].ap[:, b4 * 128:(b4 + 1) * 128],
                                          qt[tb].ap[:, b4 * 128:(b4 + 1) * 128], True, True) for b4 in range(4)]
                                mm_group(specs, [kt[par][tb].b, qt[tb].b], [SC.b])
                                tt(scm[tb].ap, SC.ap, maskBD, ALU.mult, [SC.b], [scm[tb].b])
                            late.append(scores)
                        group(w, tb, ev, nd)
                    w = wload("Aog", h)
                    for tb in range(NTB):
                        def ev(bank, tb=tb):
                            act_sigmoid(gate[tb].ap, bank.ap, sig, [bank.b], [gate[tb].b])
                        group(w, tb, ev, nd)
                    w = wload("Az", h)
                    for tb in range(NTB):
                        def ev(bank, tb=tb):
                            act_sigmoid(lf.ap, bank.ap, lf, [bank.b], [lf.b])
                            tt(sz.ap, bank.ap, lf.ap, ALU.mult, [bank.b, lf.b], [sz.b])
                            tt(gate[tb].ap, gate[tb].ap, sz.ap, ALU.mult, [sz.b, gate[tb].b], [gate[tb].b])
                        group(w, tb, ev, nd)
                    for tb in range(NTB):
                        def fin1(tb=tb):
                            vtok = vk[tb].ap[:, 0:512].rearrange("p (b v) -> p b v", b=4)
                            specs = []
                            for c in range(8):
                                cols = slice(c * 64, (c + 1) * 64)
                                specs.append((oab[tb].ap[:, cols], sball[tb].ap[:, c * 128:(c + 1) * 128],
                                              qt[tb].ap[:, cols], True, False))
                                specs.append((oab[tb].ap[:, cols], vtok[:, c // 2, :], scm[tb].ap[:, cols], False, True))
                            mm_group(specs, [sball[tb].b, qt[tb].b, vk[tb].b, scm[tb].b], [oab[tb].b])
                            act(sq[tb].ap, oab[tb].ap, AF.Square, [oab[tb].b], [sq[tb].b])
                        defer(fin1)
                    for tb in range(NTB):
                        def fin2(tb=tb, h=h, hc=hc):
                            mm_group([(MX.ap, onesb[:, :], sq[tb].ap, True, True)], [sq[tb].b], [MX.b])
                            act_rpow(rs.ap, MX.ap, [MX.b], [rs.b], 1.0 / 128, EPS, 0.5)
                            tt(tmp.ap, oab[tb].ap, rs.ap, ALU.mult, [oab[tb].b, rs.b], [tmp.b])
                            stt(oT[:, h, tb * TB:(tb + 1) * TB], tmp.ap, gna[:, hc], gate[tb].ap,
                                ALU.mult, ALU.mult, [tmp.b, gate[tb].b], [])
                        defer(fin2)
            run_late()
            flush()
            pg.barrier()

        def heads_b(main, last_pre):
            wk.reset()
            ebB = [wk.f32("ebB%d" % i, 512) for i in range(2)]
            wBb = [wk.f32("wBb%d" % i, 512) for i in range(2)]
            egB = [[wk.f32("egB%d%d" % (p_, i), 8) for i in range(2)] for p_ in range(2)]
            t0 = wk.f32("t0", 516)
            t1 = wk.f32("t1", 516)
            t2 = wk.f32("t2", 516)
            t3 = wk.f32("t3", 516)
            ktB = wk.bf("ktB", 512)
            vT = wk.bf("vT", 512)
            ktk = [wk.bf("ktk%d" % i, 512) for i in range(2)]
            vpt = [wk.bf("vpt%d" % i, 4 * 258) for i in range(2)]
            vptv = [v.ap.rearrange("p (b v) -> p b v", b=4) for v in vpt]
            ktkv = [k_.ap.rearrange("p (b v) -> p b v", b=4) for k_ in ktk]
            if main:
                qtB = [wk.bf("qtB%d" % i, 512) for i in range(2)]
                scm = [wk.bf("scmB%d" % i, 512) for i in range(2)]
                sball = [wk.bf("sballB%d" % i, 8 * 258) for i in range(2)]
                sballv = [s_.ap.rearrange("p (c v) -> p c v", c=8) for s_ in sball]
                gate = [[wk.bf("gB%d%d" % (i, j), 512) for j in range(2)] for i in range(2)]
                sz = wk.bf("szB", 512)
                sq0, sq1 = vT, sz
            for tb in range(NTB):
                pg.emit(DVE, lambda hh, a=vptv[tb][:, :, 256:258]: hh.memset(a, 1.0), [], [vpt[tb].b])
            ubanks = [UX, MX, SC, OA0] if main else [UX, MX, OA0, OA1]
            uc = [0]
            tpv = TP.ap.bitcast(BF16)

            def conv(bank, hidx, hist):
                cpy(t0.ap[:, 0:3], hist[:, hidx % 8, :], [b_hist], [t0.b])
                act(t0.ap[:, 3:515], bank.ap, AF.Copy, [bank.b], [t0.b])
                ts(t1.ap[:, 0:512], t0.ap[:, 0:512], cw[:, hidx, 0:1], ALU.mult, [t0.b], [t1.b],
                   s2=cbias[:, hidx:hidx + 1], op1=ALU.add)
                for k in range(1, 4):
                    stt(t1.ap[:, 0:512], t0.ap[:, k:k + 512], cw[:, hidx, k:k + 1], t1.ap[:, 0:512],
                        ALU.mult, ALU.add, [t0.b, t1.b], [t1.b])
                cpy(hist[:, hidx % 8, :], t0.ap[:, 512:515], [t0.b], [b_hist])

            for h in range(8):
                par = h % 2
                hc = slice(h, h + 1)
                nd0 = 1 if main else 4
                w1 = wload("Bfg", h)
                w2 = wload("Big", h)
                for tb in range(NTB):
                    def ev1(bank, tb=tb, par=par, hc=hc):
                        a0, a1 = t0.ap[:, 0:512], t1.ap[:, 0:512]
                        act(a0, bank.ap, AF.Exp, [bank.b], [t0.b], scale=-1.0, bias=nbfg[:, hc])
                        act(a0, a0, AF.Ln, [t0.b], [t0.b], bias=1.0)
                        pg.emit(DVE, lambda hh, o=a1, d1=a0: hh.tensor_tensor_scan(
                            out=o, data0=rmask, data1=d1, initial=0.0, op0=ALU.mult, op1=ALU.add),
                            [t0.b], [t1.b])
                        act(ebB[tb].ap, a1, AF.Exp, [t1.b], [ebB[tb].b], scale=-1.0)
                        act(wBb[tb].ap, a1, AF.Exp, [t1.b], [wBb[tb].b])
                        cpy(egB[par][tb].ap, ebB[tb].ap[:, 63:512:64], [ebB[tb].b], [egB[par][tb].b])
                    group(w1, tb, ev1, nd0)

                    def ev2(bank, tb=tb, hc=hc):
                        a0 = t0.ap[:, 0:512]
                        act(a0, bank.ap, AF.Exp, [bank.b], [t0.b], bias=big[:, hc])
                        tt(wBb[tb].ap, wBb[tb].ap, a0, ALU.mult, [t0.b, wBb[tb].b], [wBb[tb].b])
                    group(w2, tb, ev2, nd0)
                if main:
                    w = wload("Bq", h)
                    for tb in range(NTB):
                        def ev(bank, tb=tb, h=h):
                            conv(bank, h, histq)
                            act_sigmoid(t2.ap[:, 0:512], t1.ap[:, 0:512], Tl(t2.ap[:, 0:512], t2.b), [t1.b], [t2.b])
                            tt(t2.ap[:, 0:512], t2.ap[:, 0:512], t1.ap[:, 0:512], ALU.mult, [t1.b, t2.b], [t2.b])
                            stt(qtB[tb].ap, ebB[tb].ap, 128.0 ** -0.5, t2.ap[:, 0:512], ALU.mult, ALU.mult,
                                [ebB[tb].b, t2.b], [qtB[tb].b])
                        group(w, tb, ev, 1)
                elif last_pre:
                    w = wload("Bq", h)
                    bank = ipbank()
                    inproj(w, 1, bank)
                    cpy(histq[:, h, :], bank.ap[:, 509:512], [bank.b], [b_hist])
                run_late()
                flush()
                w = wload("Bk", h)
                for tb in range(NTB):
                    def ev(bank, tb=tb, h=h):
                        conv(bank, 8 + h, histk)
                        act_sigmoid(t2.ap[:, 0:512], t1.ap[:, 0:512], Tl(t2.ap[:, 0:512], t2.b), [t1.b], [t2.b])
                        tt(t2.ap[:, 0:512], t2.ap[:, 0:512], t1.ap[:, 0:512], ALU.mult, [t1.b, t2.b], [t2.b])
                        tt(ktB.ap, t2.ap[:, 0:512], wBb[tb].ap, ALU.mult, [t2.b, wBb[tb].b], [ktB.b])

                        def klate(tb=tb):
                            specs = [(tpv[:, b4 * 128:(b4 + 1) * 128], ktB.ap[:, b4 * 128:(b4 + 1) * 128], identB)
                                     for b4 in range(4)]
                            tr_group(specs, [ktB.b], [TP.b])
                            act(ktk[tb].ap, tpv[:, 0:512], AF.Copy, [TP.b], [ktk[tb].b])
                            if main:
                                specs = [(SC.ap[:, b4 * 128:(b4 + 1) * 128], ktB.ap[:, b4 * 128:(b4 + 1) * 128],
                                          qtB[tb].ap[:, b4 * 128:(b4 + 1) * 128], True, True) for b4 in range(4)]
                                mm_group(specs, [ktB.b, qtB[tb].b], [SC.b])
                                tt(scm[tb].ap, SC.ap, maskBD, ALU.mult, [SC.b], [scm[tb].b])
                        late.append(klate)
                    group(w, tb, ev, 0)
                for half, nm in ((0, "Bv0"), (1, "Bv1")):
                    w = wload(nm, h)
                    for tb in range(NTB):
                        def ev(bank, tb=tb, half=half):
                            act(vT.ap, bank.ap, AF.Copy, [bank.b], [vT.b])

                            def vlate(tb=tb, half=half):
                                specs = [(tpv[:, 512 + b4 * 128:512 + (b4 + 1) * 128],
                                          vT.ap[:, b4 * 128:(b4 + 1) * 128], identB) for b4 in range(4)]
                                tr_group(specs, [vT.b], [TP.b])
                                cpy(vptv[tb][:, :, half * 128:(half + 1) * 128],
                                    tpv[:, 512:1024].rearrange("p (b v) -> p b v", b=4), [TP.b], [vpt[tb].b])
                            late.append(vlate)
                        group(w, tb, ev, 0)
                for tb in range(NTB):
                    if main:
                        def sb0(tb=tb, h=h, hc=hc):
                            act(sballv[tb][:, 0, 0:257], PB[:, h, :], AF.Copy, [b_PB[h], b_pexB], [sball[tb].b],
                                scale=pexpB[:, hc])
                        defer(sb0)
                    for c in range(8):
                        def step(c=c, tb=tb, h=h, hc=hc, par=par):
                            b4 = c // 2
                            rows = slice((c % 2) * 64, (c % 2) * 64 + 64)
                            ub = ubanks[uc[0] % len(ubanks)]
                            uc[0] += 1
                            mm_group([(ub.ap[:, 0:257], ktkv[tb][rows, b4, :], vptv[tb][rows, b4, 0:257], True, True)],
                                     [ktk[tb].b, vpt[tb].b], [ub.b])
                            egt = egB[par][tb]
                            pe_ap = pexpB[:, hc] if c == 0 else egt.ap[:, c - 1:c]
                            stt(PB[:, h, :], PB[:, h, :], pe_ap, ub.ap[:, 0:257], ALU.mult, ALU.add,
                                [ub.b, egt.b, b_pexB], [b_PB[h]])
                            if c < 7:
                                if main:
                                    act(sballv[tb][:, c + 1, 0:257], PB[:, h, :], AF.Copy, [b_PB[h], egt.b],
                                        [sball[tb].b], scale=egt.ap[:, c:c + 1])
                            else:
                                cpy(pexpB[:, hc], egt.ap[:, 7:8], [egt.b], [b_pexB])
                        defer(step)
                if main:
                    for half in range(2):
                        w = wload("Bog%d" % half, h)
                        for tb in range(NTB):
                            def ev(bank, tb=tb, half=half):
                                act_sigmoid(gate[tb][half].ap, bank.ap, Tl(t3.ap[:, 0:512], t3.b), [bank.b], [gate[tb][half].b])
                            group(w, tb, ev, 3)
                        w = wload("Bz%d" % half, h)
                        for tb in range(NTB):
                            def ev(bank, tb=tb, half=half):
                                act_sigmoid(t3.ap[:, 0:512], bank.ap, Tl(t3.ap[:, 0:512], t3.b), [bank.b], [t3.b])
                                tt(sz.ap, bank.ap, t3.ap[:, 0:512], ALU.mult, [bank.b, t3.b], [sz.b])
                                tt(gate[tb][half].ap, gate[tb][half].ap, sz.ap, ALU.mult,
                                   [sz.b, gate[tb][half].b], [gate[tb][half].b])
                            group(w, tb, ev, 3)
                    for tb in range(NTB):
                        def fB1(tb=tb):
                            specs = []
                            for c in range(8):
                                b4 = c // 2
                                cols = slice(c * 64, (c + 1) * 64)
                                q_c = qtB[tb].ap[:, cols]
                                s_c2 = scm[tb].ap[:, cols]
                                specs += [(OA0.ap[:, cols], sballv[tb][:, c, 0:128], q_c, True, False),
                                          (OA0.ap[:, cols], vptv[tb][:, b4, 0:128], s_c2, False, True),
                                          (OA1.ap[:, cols], sballv[tb][:, c, 128:256], q_c, True, False),
                                          (OA1.ap[:, cols], vptv[tb][:, b4, 128:256], s_c2, False, True),
                                          (MX.ap[0:1, cols], sballv[tb][:, c, 256:257], q_c, True, False),
                                          (MX.ap[0:1, cols], onesb[:, 0:1], s_c2, False, True)]
                            mm_group(specs, [sball[tb].b, qtB[tb].b, vpt[tb].b, scm[tb].b], [OA0.b, OA1.b, MX.b])
                            r0 = t0.ap[0:1, 0:512]
                            r1 = t1.ap[0:1, 0:512]
                            act(r0, MX.ap[0:1, :], AF.Abs, [MX.b], [t0.b])
                            ts(r0, r0, 1.0, ALU.max, [t0.b], [t0.b])
                            act(r0, r0, AF.Ln, [t0.b], [t0.b])
                            act(r0, r0, AF.Exp, [t0.b], [t0.b], scale=-1.0)
                            cpy(sq0.ap[0:1, :], r0, [t0.b], [sq0.b])
                            tt(r1, r0, sq0.ap[0:1, :], ALU.subtract, [t0.b, sq0.b], [t1.b])
                            cpy(sq1.ap[0:1, :], r1, [t1.b], [sq1.b])
                        defer(fB1)

                        def fB2(tb=tb):
                            mm_group([(MX.ap, onesb[0:1, :], sq0.ap[0:1, :], True, False),
                                      (MX.ap, onesb[0:1, :], sq1.ap[0:1, :], False, True)], [sq0.b, sq1.b], [MX.b])
                            a0, a1, a2 = t0.ap[:, 0:512], t1.ap[:, 0:512], t2.ap[:, 0:512]
                            act(a0, MX.ap, AF.Copy, [MX.b], [t0.b])
                            tt(a1, OA0.ap, a0, ALU.mult, [OA0.b, t0.b], [t1.b])
                            tt(a2, OA1.ap, a0, ALU.mult, [OA1.b, t0.b], [t2.b])
                            act(sq0.ap, a1, AF.Square, [t1.b], [sq0.b])
                            act(sq1.ap, a2, AF.Square, [t2.b], [sq1.b])

                        def fB3(tb=tb, h=h):
                            a1, a2, a3 = t1.ap[:, 0:512], t2.ap[:, 0:512], t3.ap[:, 0:512]
                            mm_group([(MX.ap, onesb[:, :], sq0.ap, True, False),
                                      (MX.ap, onesb[:, :], sq1.ap, False, True)], [sq0.b, sq1.b], [MX.b])
                            act_rpow(a3, MX.ap, [MX.b], [t3.b], 1.0 / 256, EPS, 0.5)
                            for half, a in ((0, a1), (1, a2)):
                                tb_ = (t1, t2)[half]
                                tt(a, a, a3, ALU.mult, [tb_.b, t3.b], [tb_.b])
                                ch = 16 + 2 * h + half
                                stt(oT[:, ch, tb * TB:(tb + 1) * TB], a, gnb[:, 2 * h + half:2 * h + half + 1],
                                    gate[tb][half].ap, ALU.mult, ALU.mult, [tb_.b, gate[tb][half].b], [])
                        defer(lambda f2=fB2, f3=fB3: (f2(), f3()))
            run_late()
            flush()
            pg.barrier()

        def phase_b1():
            wk.reset()
            sga = [wk.f32("sga%d" % i, 512) for i in range(2)]
            sgb = [wk.f32("sgb%d" % i, 512) for i in range(2)]
            y1 = wk.f32("y1", 512)
            y2 = wk.f32("y2", 512)
            ybuf = [wk.bf("ybuf%d" % i, 1024) for i in range(2)]
            pool4 = [IP0, IP1, OA0, OA1]
            pc = 0
            b_ysc = pg.buf("ysc")
            for c in range(32):
                w = wload("Ga", c)
                for tb in range(NTB):
                    bank = pool4[pc % 4]; pc += 1
                    inproj(w, tb, bank)
                    act_sigmoid(sga[tb].ap, bank.ap, sga[tb], [bank.b], [sga[tb].b])
                w = wload("Gb", c)
                for tb in range(NTB):
                    bank = pool4[pc % 4]; pc += 1
                    inproj(w, tb, bank)
                    act_sigmoid(sgb[tb].ap, bank.ap, sgb[tb], [bank.b], [sgb[tb].b])
                w = wload("Up", c)
                yb = ybuf[c % 2]
                for tb in range(NTB):
                    ba = pool4[pc % 4]; pc += 1
                    bb_ = pool4[pc % 4]; pc += 1
                    inproj(w, tb, ba, 0, 16, src=oT)
                    inproj(w, tb, bb_, 16, 32, src=oT)
                    tt(y1.ap, ba.ap, sga[tb].ap, ALU.mult, [ba.b, sga[tb].b], [y1.b])
                    tt(y2.ap, bb_.ap, sgb[tb].ap, ALU.mult, [bb_.b, sgb[tb].b], [y2.b])
                    tt(yb.ap[:, tb * TB:(tb + 1) * TB], y1.ap, y2.ap, ALU.add, [y1.b, y2.b], [yb.b])
                dma(SP, s_ys[c % 2], ysc[c], yb.ap, [yb.b], [b_ysc])
            pg.barrier()

        def phase_b2(row0):
            yT = hT
            b_y = pg.buf("yT")
            for q4 in range(4):
                dma(SP, s_yl, yT[:, q4 * 8:(q4 + 1) * 8, :], ysc[q4 * 8:(q4 + 1) * 8].rearrange("c p t -> p c t"),
                    [], [b_y])
            ot = [Tl(R3[:, i * 1024:(i + 1) * 1024].bitcast(F32), pg.buf("ot%d" % i)) for i in range(4)]
            junk = Tl(R3[:, 4096:4608], pg.buf("junk"))
            b_sp = pg.buf("ssqp")
            b_osc = pg.buf("osc")
            pool4 = [IP0, IP1, OA0, OA1]
            pc = 0
            for cg in range(8):
                wsl = woslot[cg % 2]
                dma(POOL, s_wo[cg % 2], wsl.ap, wo[cg].rearrange("p (k j) -> p k j", k=KC), [], [wsl.b])
                for t8 in range(8):
                    bank = pool4[pc % 4]
                    o_t = ot[pc % 4]
                    s_o = s_ot[pc % 4]
                    pc += 1
                    specs = [(bank.ap, yT[:, k, t8 * P:(t8 + 1) * P], wsl.ap[:, k, :], k == 0, k == KC - 1)
                             for k in range(KC)]
                    mm_group(specs, [wsl.b, b_y], [bank.b])
                    act(o_t.ap, bank.ap, AF.Copy, [bank.b], [o_t.b])
                    act(junk.ap, bank.ap, AF.Square, [bank.b], [junk.b, b_sp], accum=ssqp[:, t8, cg:cg + 1])
                    dma(SP, s_o, osc[t8 * P:(t8 + 1) * P, cg * 512:(cg + 1) * 512], o_t.ap, [o_t.b], [b_osc])
            pg.barrier()
            gbt = Tl(R3[:, 20480:28672].bitcast(F32), pg.buf("gbt2"))
            dma(SP, s_gb, gbt.ap, gpost_d[0:1, :].partition_broadcast(P), [], [gbt.b])
            pg.emit(DVE, lambda h: h.reduce_sum(out=ssq, in_=ssqp, axis=mybir.AxisListType.X), [b_sp], [b_sp])
            act_rpow(rr, ssq, [b_sp], [b_sp], 1.0 / D, EPS, 0.5)
            ob = [Tl(R1[:, i * 8192:(i + 1) * 8192].bitcast(F32), pg.buf("ob%d" % i)) for i in range(2)]
            xb = [Tl(R1[:, (2 + i) * 8192:(3 + i) * 8192].bitcast(F32), pg.buf("xb%d" % i)) for i in range(2)]
            for t8 in range(8):
                s = t8 % 2
                dma(SP, s_ob[s], ob[s].ap, osc[t8 * P:(t8 + 1) * P, :], [], [ob[s].b])
                dma(SP, s_xb[s], xb[s].ap, xm[row0 + t8 * P:row0 + (t8 + 1) * P, :], [], [xb[s].b])
                stt(ob[s].ap, ob[s].ap, rr[:, t8:t8 + 1], gbt.ap, ALU.mult, ALU.mult, [ob[s].b, gbt.b, b_sp], [ob[s].b])
                tt(ob[s].ap, ob[s].ap, xb[s].ap, ALU.add, [ob[s].b, xb[s].b], [ob[s].b])
                dma(SP, s_st[s], y_d[row0 + t8 * P:row0 + (t8 + 1) * P, :], ob[s].ap, [ob[s].b], [])
            pg.barrier()

        class _Stop(Exception):
            pass

        def chk(name):
            if stop == name:
                pg.barrier()
                s_dbg = dsem("d_dbg")
                dma(SP, s_dbg, dbg_d[0], R1[:, :], [], [])
                dma(SP, s_dbg, dbg_d[1], R2[:, :], [], [])
                raise _Stop()

        try:
            chk("init")
            if not skip_pre:
                for seg in range(2):
                    phase_a0(xp, seg * T, gpre_d)
                    chk("pa0")
                    heads_a(False)
                    chk("pha")
                    heads_b(False, seg == 1)
                    chk("phb")
            for h in range(16):
                ts(PA[:, h, :], PA[:, h, :], flag, ALU.mult, [b_PA[h], b_prm], [b_PA[h]])
            for h in range(8):
                ts(PB[:, h, :], PB[:, h, :], flag, ALU.mult, [b_PB[h], b_prm], [b_PB[h]])
            ts(sm[:, 72:120], sm[:, 72:120], flag, ALU.mult, [b_hist, b_prm], [b_hist])
            pg.barrier()
            for seg in range(2):
                phase_a0(xm, seg * T, gpre_d)
                chk("a0")
                heads_a(True)
                chk("ha")
                heads_b(True, False)
                chk("hb")
                phase_b1()
                chk("b1")
                phase_b2(seg * T)
                chk("b2")
        except _Stop:
            pass
        pg.final_wait(SP)

        with nc.Block() as block:
            @block.tensor
            def _(e):
                for f in PE.q:
                    f(e)

            @block.scalar
            def _(e):
                for f in ACT.q:
                    f(e)

            @block.vector
            def _(e):
                for f in DVE.q:
                    f(e)

            @block.gpsimd
            def _(e):
                for f in POOL.q:
                    f(e)

            @block.sync
            def _(e):
                for f in SP.q:
                    f(e)
        counts = {e.name: len(e.q) for e in pg.engs}
    return nc, counts


def _prep_weights(w_in, w_up_a, w_up_b, w_out):
    wt = np.empty((NT, P, KC * 128), np.float32)
    w_in = np.asarray(w_in, np.float32)
    for i, (nm, h) in enumerate(TILES):
        if nm == "Up":
            a = np.asarray(w_up_a[:, h * 128:(h + 1) * 128], np.float32).reshape(16, P, 128)
            b = np.asarray(w_up_b[:, h * 128:(h + 1) * 128], np.float32).reshape(16, P, 128)
            t = np.concatenate([a, b], axis=0)
        else:
            t = w_in[:, _tile_cols(nm, h)].reshape(KC, P, 128)
        wt[i] = t.transpose(1, 0, 2).reshape(P, KC * 128)
    wo = np.empty((8, P, KC * 512), np.float32)
    w_out = np.asarray(w_out, np.float32)
    for cg in range(8):
        t = w_out[:, cg * 512:(cg + 1) * 512].reshape(KC, P, 512)
        wo[cg] = t.transpose(1, 0, 2).reshape(P, KC * 512)
    return wt, wo


def _prep_consts():
    cst = np.zeros((P, 128 + 512 + 512), np.float32)
    cst[:, 0:128] = np.eye(P, dtype=np.float32)
    t = np.arange(512)
    cst[:, 128:640] = (t % 64 != 0).astype(np.float32)[None, :]
    s = np.arange(P)[:, None]
    tt_ = np.arange(P)[None, :]
    m = ((s // 64) == (tt_ // 64)) & (s <= tt_)
    cst[:, 640:1152] = np.tile(m.astype(np.float32), (1, 4))
    return cst


def _prep_params(lb_logits, conv_w, conv_b, b_ig, b_fg, g_norm_a, g_norm_b, half):
    prm = np.zeros((P, 168), np.float32)
    col = lambda v, n: np.asarray(v, np.float32).reshape(n, P).T
    prm[:, 0:16] = col(lb_logits[0], 16)
    prm[:, 16:32] = col(lb_logits[1], 16)
    prm[:, 32:48] = col(g_norm_a, 16)
    prm[:, 48:64] = col(g_norm_b, 16)
    cwv = np.asarray(conv_w, np.float32)
    prm[:, 64:128] = cwv.reshape(4, 16, P).transpose(2, 1, 0).reshape(P, 64)
    prm[:, 128:144] = col(conv_b, 16)
    prm[:, 144:152] = np.asarray(b_ig, np.float32).reshape(1, 8)
    prm[:, 152:160] = np.asarray(b_fg, np.float32).reshape(1, 8)
    prm[:, 160] = float(half)
    return prm


_CACHE = {}


def kernel(x, g_pre, w_in, lb_logits, conv_w, conv_b, b_ig, b_fg,
           g_norm_a, g_norm_b, w_up_a, w_up_b, w_out, g_post):
    x = np.asarray(x, np.float32)
    wt, wo = _prep_weights(np.asarray(w_in)[0], np.asarray(w_up_a)[0], np.asarray(w_up_b)[0],
                           np.asarray(w_out)[0])
    cst = _prep_consts()
    gpre = np.ascontiguousarray(np.asarray(g_pre, np.float32).reshape(1, D))
    gpost = np.ascontiguousarray(np.asarray(g_post, np.float32).reshape(1, D))
    if "nc" not in _CACHE:
        _CACHE["nc"] = build_program()
    nc, _ = _CACHE["nc"]
    in_maps = []
    for c in range(8):
        b, half = c // 2, c % 2
        prm = _prep_params(np.asarray(lb_logits), np.asarray(conv_w)[0], np.asarray(conv_b)[0],
                           np.asarray(b_ig)[0], np.asarray(b_fg)[0], np.asarray(g_norm_a)[0],
                           np.asarray(g_norm_b)[0], half)
        in_maps.append({
            "xm": np.ascontiguousarray(x[b, half * NTOK:(half + 1) * NTOK]),
            "xp": np.ascontiguousarray(x[b, 0:NTOK]),
            "wt": wt, "wo": wo, "prm": prm, "cst": cst, "gpre": gpre, "gpost": gpost,
        })
    res = run_bass_kernel_spmd(nc, in_maps, core_ids=list(range(8)))
    out = np.empty((4, 4096, D), np.float32)
    for c in range(8):
        b, half = c // 2, c % 2
        out[b, half * NTOK:(half + 1) * NTOK] = res.results[c]["y"]
    return out
```

```python
import numpy as np
from collections import deque
from contextlib import ExitStack

import concourse.bass as bass
import concourse.mybir as mybir
from concourse.bass_utils import run_bass_kernel_spmd

F32 = mybir.dt.float32
BF16 = mybir.dt.bfloat16
AF = mybir.ActivationFunctionType
ALU = mybir.AluOpType

P = 128
D = 4096
KC = 32
T = 1024
TB = 512
NTB = T // TB
NTOK = 2048
EPS = 1e-6

OFF = dict(qa=0, fa=2048, ia=4096, oga=6144, za=8192, qb=10240, kb=11264, vb=12288,
           ogb=14336, zb=16384, igb=18432, fgb=18440, ga=18448, gb=22544)


def _tile_list():
    tl = []
    for h in range(16):
        for nm in ("Af", "Aq", "Ai", "Aog", "Az"):
            tl.append((nm, h))
    tl.append(("Bg", 0))
    for h in range(8):
        for nm in ("Bk", "Bq", "Bv0", "Bv1", "Bog0", "Bz0", "Bog1", "Bz1"):
            tl.append((nm, h))
    for c in range(32):
        tl.append(("Ga", c))
        tl.append(("Gb", c))
    for c in range(32):
        tl.append(("Up", c))
    for h in range(8):
        tl.append(("Bfg", h))
        tl.append(("Big", h))
    return tl


TILES = _tile_list()
TIDX = {t: i for i, t in enumerate(TILES)}
NT = len(TILES)


def _tile_cols(nm, h):
    ar = np.arange(128)
    if nm == "Af":
        return OFF["fa"] + h * 128 + ar
    if nm == "Aq":
        return OFF["qa"] + h * 128 + ar
    if nm == "Ai":
        return OFF["ia"] + h * 128 + ar
    if nm == "Aog":
        return OFF["oga"] + h * 128 + ar
    if nm == "Az":
        return OFF["za"] + h * 128 + ar
    if nm == "Bfg":
        return np.full(128, OFF["fgb"] + h)
    if nm == "Big":
        return np.full(128, OFF["igb"] + h)
    if nm == "Bg":
        return np.concatenate([OFF["igb"] + np.arange(8), OFF["fgb"] + np.arange(8), np.full(112, OFF["igb"])])
    if nm == "Bk":
        return OFF["kb"] + h * 128 + ar
    if nm == "Bq":
        return OFF["qb"] + h * 128 + ar
    if nm in ("Bv0", "Bv1"):
        return OFF["vb"] + h * 256 + (128 if nm == "Bv1" else 0) + ar
    if nm in ("Bog0", "Bog1"):
        return OFF["ogb"] + h * 256 + (128 if nm == "Bog1" else 0) + ar
    if nm in ("Bz0", "Bz1"):
        return OFF["zb"] + h * 256 + (128 if nm == "Bz1" else 0) + ar
    if nm == "Ga":
        return OFF["ga"] + h * 128 + ar
    if nm == "Gb":
        return OFF["gb"] + h * 128 + ar
    raise KeyError(nm)


class Sem:
    def __init__(self, h, step):
        self.h = h
        self.step = step
        self.n = 0


class Eng:
    def __init__(self, name, sem):
        self.name = name
        self.sem = sem
        self.q = []
        self.waited = {}


class Buf:
    __slots__ = ("name", "w", "r")

    def __init__(self, name):
        self.name = name
        self.w = None
        self.r = {}


class Prog:
    def __init__(self, nc, es):
        self.nc = nc
        self.es = es
        self.sems = []
        self.bufs = []
        mk = lambda nm, st: self.new_sem(nm, st)
        self.PE = Eng("tensor", mk("s_pe", 1))
        self.ACT = Eng("scalar", mk("s_act", 1))
        self.DVE = Eng("vector", mk("s_dve", 1))
        self.POOL = Eng("gpsimd", mk("s_pool", 1))
        self.SP = Eng("sync", mk("s_sp", 1))
        self.engs = [self.PE, self.ACT, self.DVE, self.POOL, self.SP]

    def new_sem(self, name, step):
        s = Sem(self.es.enter_context(self.nc.semaphore(name)), step)
        self.sems.append(s)
        return s

    def buf(self, name):
        b = Buf(name)
        self.bufs.append(b)
        return b

    def _wait(self, eng, sem, val, raw=False):
        if sem is eng.sem and not raw:
            return
        if eng.waited.get(sem, 0) >= val:
            return
        eng.waited[sem] = val
        eng.q.append(lambda h, s=sem.h, v=val: h.wait_ge(s, v))

    def emit(self, eng, fn, reads=(), writes=(), sem=None):
        for b in reads:
            if b.w is not None:
                self._wait(eng, b.w[0], b.w[1], raw=True)
        for b in writes:
            if b.w is not None:
                self._wait(eng, b.w[0], b.w[1])
            for s, v in b.r.items():
                self._wait(eng, s, v)
        s = sem if sem is not None else eng.sem
        s.n += 1
        val = s.n * s.step
        eng.q.append(lambda h, f=fn, sh=s.h, st=s.step: f(h).then_inc(sh, st))
        tok = (s, val)
        for b in reads:
            if b.r.get(s, 0) < val:
                b.r[s] = val
        for b in writes:
            b.w = tok
            b.r = {}
        return tok

    def barrier(self):
        for e in self.engs:
            for s in self.sems:
                if s.n > 0:
                    self._wait(e, s, s.n * s.step)
        for b in self.bufs:
            b.w = None
            b.r = {}

    def final_wait(self, eng):
        for s in self.sems:
            if s.n > 0:
                self._wait(eng, s, s.n * s.step)


class Tl:
    __slots__ = ("ap", "b")

    def __init__(self, ap, b):
        self.ap = ap
        self.b = b


def build_program(stop=None, skip_pre=False, nt=NT, nwo=8):
    nc = bass.Bass("TRN2", target_bir_lowering=False)
    xm = nc.dram_tensor("xm", [NTOK, D], F32, kind="ExternalInput").ap()
    xp = nc.dram_tensor("xp", [NTOK, D], F32, kind="ExternalInput").ap()
    wt = nc.dram_tensor("wt", [nt, P, KC * 128], F32, kind="ExternalInput").ap()
    wo = nc.dram_tensor("wo", [nwo, P, KC * 512], F32, kind="ExternalInput").ap()
    prm_d = nc.dram_tensor("prm", [P, 168], F32, kind="ExternalInput").ap()
    cst_d = nc.dram_tensor("cst", [P, 128 + 512 + 512], F32, kind="ExternalInput").ap()
    gpre_d = nc.dram_tensor("gpre", [1, D], F32, kind="ExternalInput").ap()
    gpost_d = nc.dram_tensor("gpost", [1, D], F32, kind="ExternalInput").ap()
    y_d = nc.dram_tensor("y", [NTOK, D], F32, kind="ExternalOutput").ap()
    dbg_d = nc.dram_tensor("dbg", [2, P, 32768], BF16, kind="ExternalOutput").ap() if stop else None
    ysc = nc.dram_tensor("ysc", [32, P, T], BF16).ap()
    osc = nc.dram_tensor("osc", [T, D], F32).ap()
    gsc = nc.dram_tensor("gsc", [2, 8, T], F32).ap()

    with ExitStack() as es:
        pg = Prog(nc, es)
        PE, ACT, DVE, POOL, SP = pg.PE, pg.ACT, pg.DVE, pg.POOL, pg.SP
        sb = lambda nm, shape, dt: es.enter_context(nc.sbuf_tensor(nm, shape, dt))
        R1 = sb("R1", [P, 32768], BF16)
        R2 = sb("R2", [P, 32768], BF16)
        R3 = sb("R3", [P, 29696], BF16)
        PA = sb("PA", [P, 16, 128], F32)
        PB = sb("PB", [P, 8, 257], F32)
        sm = sb("sm", [P, 256], F32)
        prm = sb("prm_s", [P, 168], F32)
        cF = sb("cF", [P, 128 + 512], F32)
        cB = sb("cB", [P, 128 + 512], BF16)
        onesb = sb("onesb", [P, 128], BF16)
        banks = [es.enter_context(nc.psum_tensor("pb%d" % i, [P, 512], F32)) for i in range(8)]
        bk = [Tl(b[:, :], pg.buf("pb%d" % i)) for i, b in enumerate(banks)]
        IP0, IP1, TP, SC, OA0, OA1, UX, MX = bk

        l0 = prm[:, 0:16]
        l1 = prm[:, 16:32]
        gna = prm[:, 32:48]
        gnb = prm[:, 48:64]
        cw = prm[:, 64:128].rearrange("p (h k) -> p h k", k=4)
        cbias = prm[:, 128:144]
        big = prm[:, 144:152]
        bfg = prm[:, 152:160]
        flag = prm[:, 160:161]
        lb = sm[:, 0:16]
        oml = sm[:, 16:32]
        noml = sm[:, 32:48]
        pexpA = sm[:, 48:64]
        pexpB = sm[:, 64:72]
        histq = sm[:, 72:96].rearrange("p (h k) -> p h k", k=3)
        histk = sm[:, 96:120].rearrange("p (h k) -> p h k", k=3)
        ssq = sm[:, 120:128]
        rr = sm[:, 128:136]
        ssqp = sm[:, 136:200].rearrange("p (t c) -> p t c", c=8)
        nbfg = sm[:, 200:208]
        nbfgc = sm[:, 208:209]
        bigc = prm[:, 162:163]
        identF = cF[:, 0:128]
        rmask = cF[:, 128:640]
        identB = cB[:, 0:128]
        maskBD = cB[:, 128:640]

        hT = R1[:, :].rearrange("p (k t) -> p k t", k=KC)
        oT = R2[:, :].rearrange("p (k t) -> p k t", k=KC)
        woslot = [Tl(oT[:, :, s * 512:(s + 1) * 512], pg.buf("wo%d" % s)) for s in range(2)]

        dsem = lambda nm: pg.new_sem(nm, 16)
        s_c = dsem("d_c")
        s_x = [dsem("d_x0"), dsem("d_x1")]
        s_w = [dsem("d_w0"), dsem("d_w1")]
        s_wo = [dsem("d_wo0"), dsem("d_wo1")]
        s_gb = dsem("d_gb")
        s_ys = [dsem("d_ys0"), dsem("d_ys1")]
        s_yl = dsem("d_yl")
        s_ot = [dsem("d_ot%d" % i) for i in range(4)]
        s_ob = [dsem("d_ob0"), dsem("d_ob1")]
        s_xb = [dsem("d_xb0"), dsem("d_xb1")]
        s_st = [dsem("d_st0"), dsem("d_st1")]
        s_gw = dsem("d_gw")
        s_ge = [dsem("d_ge0"), dsem("d_ge1")]
        s_gl = dsem("d_gl")

        def act(out, in_, func, reads, writes, scale=1.0, bias=0.0, accum=None):
            def f(h):
                if accum is None:
                    return h.activation(out=out, in_=in_, func=func, bias=bias, scale=scale)
                return h.activation(out=out, in_=in_, func=func, bias=bias, scale=scale,
                                    accum_out=accum)
            return pg.emit(ACT, f, reads, writes)

        def tt(out, in0, in1, op, reads, writes, eng=None):
            return pg.emit(eng or DVE, lambda h: h.tensor_tensor(out=out, in0=in0, in1=in1, op=op),
                           reads, writes)

        def ts(out, in0, s1, op0, reads, writes, s2=None, op1=None, eng=None):
            def f(h):
                if op1 is None:
                    return h.tensor_scalar(out=out, in0=in0, scalar1=s1, scalar2=None, op0=op0)
                return h.tensor_scalar(out=out, in0=in0, scalar1=s1, scalar2=s2, op0=op0, op1=op1)
            return pg.emit(eng or DVE, f, reads, writes)

        def stt(out, in0, scalar, in1, op0, op1, reads, writes):
            return pg.emit(DVE, lambda h: h.scalar_tensor_tensor(out=out, in0=in0, scalar=scalar,
                                                                  in1=in1, op0=op0, op1=op1),
                           reads, writes)

        def act_sigmoid(out, in_, tmp, reads, writes, scale=1.0, bias=0.0):
            act(tmp.ap if isinstance(tmp, Tl) else tmp, in_, AF.Exp, reads, [tmp.b], scale=-scale, bias=bias)
            act(tmp.ap, tmp.ap, AF.Ln, [tmp.b], [tmp.b], bias=1.0)
            return act(out, tmp.ap, AF.Exp, [tmp.b], writes, scale=-1.0)

        def act_rpow(out, in_, reads, writes, scale, bias, p):
            act(out, in_, AF.Ln, reads, writes, scale=scale, bias=bias)
            return act(out, out, AF.Exp, writes, writes, scale=-p)

        def cpy(out, in_, reads, writes, eng=None):
            return pg.emit(eng or DVE, lambda h: h.tensor_copy(out=out, in_=in_), reads, writes)

        def recip(out, in_, reads, writes):
            return pg.emit(DVE, lambda h: h.reciprocal(out=out, in_=in_), reads, writes)

        def dma(eng, sem, out, in_, reads, writes):
            return pg.emit(eng, lambda h: h.dma_start(out=out, in_=in_), reads, writes, sem=sem)

        def mm_group(specs, reads, writes):
            def f(h):
                ins = None
                for (o, l, r, st, sp) in specs:
                    ins = h.matmul(o, l, r, start=st, stop=sp)
                return ins
            return pg.emit(PE, f, reads, writes)

        def tr_group(specs, reads, writes):
            def f(h):
                ins = None
                for (o, i, idn) in specs:
                    ins = h.transpose(o, i, idn)
                return ins
            return pg.emit(PE, f, reads, writes)

        b_prm = pg.buf("prm")
        b_c = pg.buf("consts")
        b_sm = pg.buf("sm")
        b_PA = [pg.buf("PA%d" % h) for h in range(16)]
        b_PB = [pg.buf("PB%d" % h) for h in range(8)]
        dma(SP, s_c, prm[:, :], prm_d[:, :], [], [b_prm])
        dma(SP, s_c, cF[:, :], cst_d[:, 0:640], [], [b_c])
        dma(POOL, s_c, cB[:, 0:128], cst_d[:, 0:128], [], [b_c])
        dma(POOL, s_c, cB[:, 128:640], cst_d[:, 640:1152], [], [b_c])
        pg.emit(DVE, lambda h: h.memset(onesb[:, :], 1.0), [], [b_c])
        pg.emit(DVE, lambda h: h.memset(sm[:, :], 0.0), [], [b_sm])
        pg.emit(DVE, lambda h: h.memset(PA[:, :, :], 0.0), [], b_PA)
        pg.emit(DVE, lambda h: h.memset(PB[:, :, :], 0.0), [], b_PB)
        tt(lb, l0, l1, ALU.subtract, [b_prm], [b_sm])
        act(lb, lb, AF.Exp, [b_sm], [b_sm], scale=-1.0)
        act(lb, lb, AF.Ln, [b_sm], [b_sm], bias=1.0)
        act(lb, lb, AF.Exp, [b_sm], [b_sm], scale=-1.0)
        ts(nbfg, bfg, -1.0, ALU.mult, [b_prm], [b_sm])
        ts(nbfgc, prm[:, 161:162], -1.0, ALU.mult, [b_prm], [b_sm])
        ts(oml, lb, -1.0, ALU.mult, [b_sm], [b_sm], s2=1.0, op1=ALU.add)
        ts(noml, lb, -1.0, ALU.add, [b_sm], [b_sm])
        pg.barrier()

        wslot = [Tl(R3[:, i * 4096:(i + 1) * 4096].rearrange("p (k j) -> p k j", k=KC),
                    pg.buf("ws%d" % i)) for i in range(2)]
        wctr = [0]

        def wload(name, idx):
            s = wctr[0] % 2
            wctr[0] += 1
            t = wslot[s]
            dma(POOL, s_w[s], t.ap, wt[TIDX[(name, idx)]].rearrange("p (k j) -> p k j", k=KC),
                [], [t.b])
            return t

        ipc = [0]

        def ipbank():
            b = (IP0, IP1)[ipc[0] % 2]
            ipc[0] += 1
            return b

        def inproj(w, tb, bank, k0=0, k1=KC, src=None):
            src = hT if src is None else src
            specs = [(bank.ap, w.ap[:, k, :], src[:, k, tb * TB:(tb + 1) * TB], k == k0, k == k1 - 1)
                     for k in range(k0, k1)]
            return mm_group(specs, [w.b], [bank.b])

        class Work:
            def __init__(self):
                self.off = 8192

            def reset(self):
                self.off = 8192

            def bf(self, name, n):
                a = R3[:, self.off:self.off + n]
                self.off += n + (n % 2)
                assert self.off <= 29696, "work overflow"
                return Tl(a, pg.buf(name))

            def f32(self, name, n):
                a = R3[:, self.off:self.off + 2 * n].bitcast(F32)
                self.off += 2 * n
                assert self.off <= 29696, "work overflow"
                return Tl(a, pg.buf(name))

        wk = Work()

        def phase_a0(xsrc, row0, gb_src):
            xt = [Tl(R3[:, i * 8192:(i + 1) * 8192].bitcast(F32), pg.buf("xt%d" % i)) for i in range(2)]
            xsl = [Tl(R2[:, i * 4096:(i + 1) * 4096], pg.buf("xs%d" % i)) for i in range(2)]
            junk = Tl(R2[:, 8192:12288], pg.buf("junkA0"))
            gbt = Tl(R3[:, 20480:28672].bitcast(F32), pg.buf("gbt"))
            b_ss = pg.buf("ssq")
            dma(SP, s_gb, gbt.ap, gb_src[0:1, :].partition_broadcast(P), [], [gbt.b])
            tpb = [IP0, IP1, TP, SC]
            for t8 in range(T // P):
                x = xt[t8 % 2]
                xs = xsl[t8 % 2]
                dma(SP, s_x[t8 % 2], x.ap, xsrc[row0 + t8 * P: row0 + (t8 + 1) * P, :], [], [x.b])
                act(junk.ap, x.ap, AF.Square, [x.b], [junk.b, b_ss], accum=ssq[:, t8:t8 + 1])
                act_rpow(rr[:, t8:t8 + 1], ssq[:, t8:t8 + 1], [b_ss], [b_ss], 1.0 / D, EPS, 0.5)
                stt(xs.ap, x.ap, rr[:, t8:t8 + 1], gbt.ap, ALU.mult, ALU.mult, [x.b, gbt.b, b_ss], [xs.b])
                for g in range(4):
                    bank = tpb[g]
                    bv = bank.ap.bitcast(BF16)
                    specs = [(bv[:, i * 128:(i + 1) * 128], xs.ap[:, (g * 8 + i) * 128:(g * 8 + i + 1) * 128], identB)
                             for i in range(8)]
                    tr_group(specs, [xs.b], [bank.b])
                    dst = hT[:, g * 8:(g + 1) * 8, t8 * P:(t8 + 1) * P]
                    srcv = bv.rearrange("p (k t) -> p k t", k=8)
                    if g % 2 == 0:
                        act(dst, srcv, AF.Copy, [bank.b], [])
                    else:
                        cpy(dst, srcv, [bank.b], [])
            pg.barrier()

        pend = deque()
        late = []

        def defer(fn):
            pend.append(fn)

        def drain(k):
            while k > 0 and pend:
                pend.popleft()()
                k -= 1

        def flush():
            while pend:
                pend.popleft()()

        def run_late():
            while late:
                late.pop(0)()

        def group(w, tb, evac, ndrain):
            drain(ndrain)
            bank = ipbank()
            inproj(w, tb, bank)
            run_late()
            evac(bank)

        b_pexA = pg.buf("pexA")
        b_pexB = pg.buf("pexB")
        b_hist = pg.buf("hist")

        def heads_a(main):
            wk.reset()
            sig = wk.f32("sig", 512)
            lf = wk.f32("lf", 512)
            bb = wk.f32("bb", 512)
            e1 = [wk.f32("e1_%d" % i, 512) for i in range(2)]
            eg = [[wk.f32("eg%d%d" % (p_, i), 8) for i in range(2)] for p_ in range(2)]
            kt = [[wk.bf("kt%d%d" % (p_, i), 512) for i in range(2)] for p_ in range(2)]
            it = [wk.bf("it%d" % i, 512) for i in range(2)]
            vk = [wk.bf("vk%d" % i, 1024) for i in range(2)]
            if main:
                qt = [wk.bf("qt%d" % i, 512) for i in range(2)]
                gate = [wk.bf("gate%d" % i, 512) for i in range(2)]
                sz = wk.bf("sz", 512)
                scm = [wk.bf("scm%d" % i, 512) for i in range(2)]
                sball = [wk.bf("sball%d" % i, 1024) for i in range(2)]
                sq = [wk.bf("sq%d" % i, 512) for i in range(2)]
                rs = wk.f32("rs", 512)
                tmp = wk.f32("tmp", 512)
                oab = [OA0, OA1]
            ubanks = [UX, TP, MX] if main else [UX, SC, MX, OA0]
            uc = [0]
            nd = 3
            nd_f = 1 if main else 4
            for h in range(16):
                par = h % 2
                hc = slice(h, h + 1)
                w = wload("Af", h)
                for tb in range(NTB):
                    def ev(bank, tb=tb):
                        act(sig.ap, bank.ap, AF.Exp, [bank.b], [sig.b], scale=-1.0)
                        act(lf.ap, sig.ap, AF.Ln, [sig.b], [lf.b], bias=1.0)
                        act(sig.ap, lf.ap, AF.Exp, [lf.b], [sig.b], scale=-1.0)
                        act(lf.ap, sig.ap, AF.Ln, [sig.b], [lf.b], scale=oml[:, hc], bias=lb[:, hc])
                        pg.emit(DVE, lambda hh, o=bb.ap, d1=lf.ap: hh.tensor_tensor_scan(
                            out=o, data0=rmask, data1=d1, initial=0.0, op0=ALU.mult, op1=ALU.add),
                            [lf.b], [bb.b])
                        act(e1[tb].ap, bb.ap, AF.Exp, [bb.b], [e1[tb].b])
                        act(lf.ap, bb.ap, AF.Exp, [bb.b], [lf.b], scale=-1.0)
                        stt(kt[par][tb].ap, sig.ap, -1.0, lf.ap, ALU.add, ALU.mult, [sig.b, lf.b], [kt[par][tb].b])
                        cpy(eg[par][tb].ap, e1[tb].ap[:, 63:512:64], [e1[tb].b], [eg[par][tb].b])
                    group(w, tb, ev, nd_f)
                w = wload("Ai", h)
                for tb in range(NTB):
                    def ev(bank, tb=tb):
                        act(it[tb].ap, bank.ap, AF.Copy, [bank.b], [it[tb].b])
                    group(w, tb, ev, nd_f)
                flush()
                for tb in range(NTB):
                    tpv = TP.ap.bitcast(BF16)
                    specs = [(tpv[:, b4 * 128:(b4 + 1) * 128], it[tb].ap[:, b4 * 128:(b4 + 1) * 128], identB)
                             for b4 in range(4)]
                    specs += [(tpv[:, 512 + b4 * 128:512 + (b4 + 1) * 128],
                               kt[par][tb].ap[:, b4 * 128:(b4 + 1) * 128], identB) for b4 in range(4)]
                    tr_group(specs, [it[tb].b, kt[par][tb].b], [TP.b])
                    act(vk[tb].ap, tpv, AF.Copy, [TP.b], [vk[tb].b])
                for tb in range(NTB):
                    vtok = vk[tb].ap[:, 0:512].rearrange("p (b v) -> p b v", b=4)
                    ktok = vk[tb].ap[:, 512:1024].rearrange("p (b v) -> p b v", b=4)
                    if main:
                        def sb0(tb=tb, h=h, hc=hc):
                            act(sball[tb].ap[:, 0:128], PA[:, h, :], AF.Copy, [b_PA[h], b_pexA], [sball[tb].b],
                                scale=pexpA[:, hc])
                        defer(sb0)
                    for c in range(8):
                        def step(c=c, tb=tb, h=h, hc=hc, par=par, vtok=vtok, ktok=ktok):
                            b4 = c // 2
                            rows = slice((c % 2) * 64, (c % 2) * 64 + 64)
                            ub = ubanks[uc[0] % len(ubanks)]
                            uc[0] += 1
                            mm_group([(ub.ap[:, 0:128], ktok[rows, b4, :], vtok[rows, b4, :], True, True)],
                                     [vk[tb].b], [ub.b])
                            egt = eg[par][tb]
                            pe_ap = pexpA[:, hc] if c == 0 else egt.ap[:, c - 1:c]
                            stt(PA[:, h, :], PA[:, h, :], pe_ap, ub.ap[:, 0:128], ALU.mult, ALU.add,
                                [ub.b, egt.b, b_pexA], [b_PA[h]])
                            if c < 7:
                                if main:
                                    act(sball[tb].ap[:, (c + 1) * 128:(c + 2) * 128], PA[:, h, :], AF.Copy,
                                        [b_PA[h], egt.b], [sball[tb].b], scale=egt.ap[:, c:c + 1])
                            else:
                                cpy(pexpA[:, hc], egt.ap[:, 7:8], [egt.b], [b_pexA])
                        defer(step)
                if main:
                    w = wload("Aq", h)
                    for tb in range(NTB):
                        def ev(bank, tb=tb, par=par):
                            stt(qt[tb].ap, bank.ap, noml[:, hc], e1[tb].ap, ALU.mult, ALU.mult,
                                [bank.b, e1[tb].b], [qt[tb].b])

                            def scores(tb=tb, par=par):
                                specs = [(SC.ap[:, b4 * 128:(b4 + 1) * 128], kt[par][tb].ap[:, b4 * 128:(b4 + 1) * 128],
                                          qt[tb].ap[:, b4 * 128:(b4 + 1) * 128], True, True) for b4 in range(4)]
                                mm_group(specs, [kt[par][tb].b, qt[tb].b], [SC.b])
                                tt(scm[tb].ap, SC.ap, maskBD, ALU.mult, [SC.b], [scm[tb].b])
                            late.append(scores)
                        group(w, tb, ev, nd)
                    w = wload("Aog", h)
                    for tb in range(NTB):
                        def ev(bank, tb=tb):
                            act_sigmoid(gate[tb].ap, bank.ap, sig, [bank.b], [gate[tb].b])
                        group(w, tb, ev, nd)
                    w = wload("Az", h)
                    for tb in range(NTB):
                        def ev(bank, tb=tb):
                            act_sigmoid(lf.ap, bank.ap, lf, [bank.b], [lf.b])
                            tt(sz.ap, bank.ap, lf.ap, ALU.mult, [bank.b, lf.b], [sz.b])
                            tt(gate[tb].ap, gate[tb].ap, sz.ap, ALU.mult, [sz.b, gate[tb].b], [gate[tb].b])
                        group(w, tb, ev, nd)
                    for tb in range(NTB):
                        def fin1(tb=tb):
                            vtok = vk[tb].ap[:, 0:512].rearrange("p (b v) -> p b v", b=4)
                            specs = []
                            for c in range(8):
                                cols = slice(c * 64, (c + 1) * 64)
                                specs.append((oab[tb].ap[:, cols], sball[tb].ap[:, c * 128:(c + 1) * 128],
                                              qt[tb].ap[:, cols], True, False))
                                specs.append((oab[tb].ap[:, cols], vtok[:, c // 2, :], scm[tb].ap[:, cols], False, True))
                            mm_group(specs, [sball[tb].b, qt[tb].b, vk[tb].b, scm[tb].b], [oab[tb].b])
                            act(sq[tb].ap, oab[tb].ap, AF.Square, [oab[tb].b], [sq[tb].b])
                        defer(fin1)
                    for tb in range(NTB):
                        def fin2(tb=tb, h=h, hc=hc):
                            mm_group([(MX.ap, onesb[:, :], sq[tb].ap, True, True)], [sq[tb].b], [MX.b])
                            act_rpow(rs.ap, MX.ap, [MX.b], [rs.b], 1.0 / 128, EPS, 0.5)
                            tt(tmp.ap, oab[tb].ap, rs.ap, ALU.mult, [oab[tb].b, rs.b], [tmp.b])
                            stt(oT[:, h, tb * TB:(tb + 1) * TB], tmp.ap, gna[:, hc], gate[tb].ap,
                                ALU.mult, ALU.mult, [tmp.b, gate[tb].b], [])
                        defer(fin2)
            run_late()
            flush()
            pg.barrier()

        def heads_b(main, last_pre):
            wk.reset()
            ebB = [wk.f32("ebB%d" % i, 512) for i in range(2)]
            wBb = wk.f32("wBb", 512)
            egB = [[wk.f32("egB%d%d" % (p_, i), 8) for i in range(2)] for p_ in range(2)]
            t0 = wk.f32("t0", 516)
            t1 = wk.f32("t1", 516)
            t2 = wk.f32("t2", 516)
            t3 = wk.f32("t3", 516)
            ktB = wk.bf("ktB", 512)
            vT = wk.bf("vT", 512)
            npar = 1 if main else 2
            ktk = [[wk.bf("ktk%d%d" % (p_, i), 512) for i in range(2)] for p_ in range(npar)]
            vpt = [[wk.bf("vpt%d%d" % (p_, i), 4 * 258) for i in range(2)] for p_ in range(npar)]
            vptv = [[v.ap.rearrange("p (b v) -> p b v", b=4) for v in vp] for vp in vpt]
            ktkv = [[k_.ap.rearrange("p (b v) -> p b v", b=4) for k_ in kp] for kp in ktk]
            if main:
                qtB = [[wk.bf("qtB%d%d" % (p_, i), 512) for i in range(2)] for p_ in range(2)]
                scm = [wk.bf("scmB%d" % i, 512) for i in range(2)]
                sball = [wk.bf("sballB%d" % i, 8 * 258) for i in range(2)]
                sballv = [s_.ap.rearrange("p (c v) -> p c v", c=8) for s_ in sball]
                gate = [[wk.bf("gB%d%d" % (i, j), 512) for j in range(2)] for i in range(2)]
                sz = wk.bf("szB", 512)
                sq0, sq1 = vT, sz
            for pp_ in range(npar):
                for tb in range(NTB):
                    pg.emit(DVE, lambda hh, a=vptv[pp_][tb][:, :, 256:258]: hh.memset(a, 1.0), [], [vpt[pp_][tb].b])
            ubanks = [UX, MX, SC, OA0] if main else [UX, MX, OA0, OA1]
            uc = [0]
            tpv = TP.ap.bitcast(BF16)
            b_gsc = pg.buf("gsc")

            w = wload("Bg", 0)
            r8 = lambda t: t.ap[0:8, 0:512]
            for tb in range(NTB):
                tbc = slice(tb * TB, (tb + 1) * TB)
                bA = ipbank()
                mm_group([(bA.ap[0:8, :], w.ap[:, k, 0:8], hT[:, k, tbc], k == 0, k == KC - 1) for k in range(KC)],
                         [w.b], [bA.b])
                bB = ipbank()
                mm_group([(bB.ap[0:8, :], w.ap[:, k, 8:16], hT[:, k, tbc], k == 0, k == KC - 1) for k in range(KC)],
                         [w.b], [bB.b])
                act(r8(t0), bB.ap[0:8, :], AF.Exp, [bB.b, b_sm], [t0.b], scale=-1.0, bias=nbfgc[0:8, :])
                act(r8(t0), r8(t0), AF.Ln, [t0.b], [t0.b], bias=1.0)
                pg.emit(DVE, lambda hh, o=r8(t1), d1=r8(t0): hh.tensor_tensor_scan(
                    out=o, data0=rmask[0:8, :], data1=d1, initial=0.0, op0=ALU.mult, op1=ALU.add),
                    [t0.b], [t1.b])
                act(r8(t2), r8(t1), AF.Exp, [t1.b], [t2.b], scale=-1.0)
                act(r8(t3), r8(t1), AF.Exp, [t1.b], [t3.b])
                act(r8(t0), bA.ap[0:8, :], AF.Exp, [bA.b, b_prm], [t0.b], bias=bigc[0:8, :])
                tt(r8(t3), r8(t3), r8(t0), ALU.mult, [t0.b, t3.b], [t3.b])
                dma(SP, s_gw, gsc[0, :, tbc], r8(t2), [t2.b], [b_gsc])
                dma(SP, s_gw, gsc[1, :, tbc], r8(t3), [t3.b], [b_gsc])

            def conv(bank, hidx, hist):
                cpy(t0.ap[:, 0:3], hist[:, hidx % 8, :], [b_hist], [t0.b])
                act(t0.ap[:, 3:515], bank.ap, AF.Copy, [bank.b], [t0.b])
                ts(t1.ap[:, 0:512], t0.ap[:, 0:512], cw[:, hidx, 0:1], ALU.mult, [t0.b], [t1.b],
                   s2=cbias[:, hidx:hidx + 1], op1=ALU.add)
                for k in range(1, 4):
                    stt(t1.ap[:, 0:512], t0.ap[:, k:k + 512], cw[:, hidx, k:k + 1], t1.ap[:, 0:512],
                        ALU.mult, ALU.add, [t0.b, t1.b], [t1.b])
                cpy(hist[:, hidx % 8, :], t0.ap[:, 512:515], [t0.b], [b_hist])

            def silu_t1_to_t2():
                a1, a2 = t1.ap[:, 0:512], t2.ap[:, 0:512]
                act_sigmoid(a2, a1, Tl(a2, t2.b), [t1.b], [t2.b])
                tt(a2, a2, a1, ALU.mult, [t1.b, t2.b], [t2.b])

            for h in range(8):
                par = h % 2
                pp = 0 if main else par
                hc = slice(h, h + 1)
                kk, kkv, vv, vvv = ktk[pp], ktkv[pp], vpt[pp], vptv[pp]
                for tb in range(NTB):
                    dma(SP, s_ge[tb], ebB[tb].ap, gsc[0, h:h + 1, tb * TB:(tb + 1) * TB].partition_broadcast(P),
                        [b_gsc], [ebB[tb].b])
                    cpy(egB[par][tb].ap, ebB[tb].ap[:, 63:512:64], [ebB[tb].b], [egB[par][tb].b])
                dma(SP, s_gl, wBb.ap, gsc[1, h:h + 1, 0:TB].partition_broadcast(P), [b_gsc], [wBb.b])
                if main:
                    w = wload("Bq", h)
                    for tb in range(NTB):
                        def ev(bank, tb=tb, h=h, par=par):
                            conv(bank, h, histq)
                            silu_t1_to_t2()
                            stt(qtB[par][tb].ap, ebB[tb].ap, 128.0 ** -0.5, t2.ap[:, 0:512], ALU.mult, ALU.mult,
                                [ebB[tb].b, t2.b], [qtB[par][tb].b])
                        group(w, tb, ev, 1)
                elif last_pre:
                    w = wload("Bq", h)
                    bank = ipbank()
                    inproj(w, 1, bank)
                    cpy(histq[:, h, :], bank.ap[:, 509:512], [bank.b], [b_hist])
                w = wload("Bk", h)
                for tb in range(NTB):
                    def ev(bank, tb=tb, h=h, par=par, kk=kk):
                        conv(bank, 8 + h, histk)
                        silu_t1_to_t2()
                        tt(ktB.ap, t2.ap[:, 0:512], wBb.ap, ALU.mult, [t2.b, wBb.b], [ktB.b])
                        if tb == 0:
                            dma(SP, s_gl, wBb.ap, gsc[1, h:h + 1, TB:2 * TB].partition_broadcast(P), [b_gsc], [wBb.b])

                        def klate(tb=tb, par=par, kk=kk):
                            specs = [(tpv[:, b4 * 128:(b4 + 1) * 128], ktB.ap[:, b4 * 128:(b4 + 1) * 128], identB)
                                     for b4 in range(4)]
                            tr_group(specs, [ktB.b], [TP.b])
                            act(kk[tb].ap, tpv[:, 0:512], AF.Copy, [TP.b], [kk[tb].b])
                            if main:
                                specs = [(SC.ap[:, b4 * 128:(b4 + 1) * 128], ktB.ap[:, b4 * 128:(b4 + 1) * 128],
                                          qtB[par][tb].ap[:, b4 * 128:(b4 + 1) * 128], True, True) for b4 in range(4)]
                                mm_group(specs, [ktB.b, qtB[par][tb].b], [SC.b])
                                tt(scm[tb].ap, SC.ap, maskBD, ALU.mult, [SC.b], [scm[tb].b])
                        late.append(klate)
                    group(w, tb, ev, 1 if main else 3)
                if main:
                    flush()
                for half, nm in ((0, "Bv0"), (1, "Bv1")):
                    w = wload(nm, h)
                    for tb in range(NTB):
                        def ev(bank, tb=tb, half=half, vv=vv, vvv=vvv):
                            act(vT.ap, bank.ap, AF.Copy, [bank.b], [vT.b])

                            def vlate(tb=tb, half=half, vv=vv, vvv=vvv):
                                specs = [(tpv[:, 512 + b4 * 128:512 + (b4 + 1) * 128],
                                          vT.ap[:, b4 * 128:(b4 + 1) * 128], identB) for b4 in range(4)]
                                tr_group(specs, [vT.b], [TP.b])
                                cpy(vvv[tb][:, :, half * 128:(half + 1) * 128],
                                    tpv[:, 512:1024].rearrange("p (b v) -> p b v", b=4), [TP.b], [vv[tb].b])
                            late.append(vlate)
                        group(w, tb, ev, 0 if main else 3)
                if main:
                    flush()
                for tb in range(NTB):
                    if main:
                        def sb0(tb=tb, h=h, hc=hc):
                            act(sballv[tb][:, 0, 0:257], PB[:, h, :], AF.Copy, [b_PB[h], b_pexB], [sball[tb].b],
                                scale=pexpB[:, hc])
                        defer(sb0)
                    for c in range(8):
                        def step(c=c, tb=tb, h=h, hc=hc, par=par, kk=kk, kkv=kkv, vv=vv, vvv=vvv):
                            b4 = c // 2
                            rows = slice((c % 2) * 64, (c % 2) * 64 + 64)
                            ub = ubanks[uc[0] % len(ubanks)]
                            uc[0] += 1
                            mm_group([(ub.ap[:, 0:257], kkv[tb][rows, b4, :], vvv[tb][rows, b4, 0:257], True, True)],
                                     [kk[tb].b, vv[tb].b], [ub.b])
                            egt = egB[par][tb]
                            pe_ap = pexpB[:, hc] if c == 0 else egt.ap[:, c - 1:c]
                            stt(PB[:, h, :], PB[:, h, :], pe_ap, ub.ap[:, 0:257], ALU.mult, ALU.add,
                                [ub.b, egt.b, b_pexB], [b_PB[h]])
                            if c < 7:
                                if main:
                                    act(sballv[tb][:, c + 1, 0:257], PB[:, h, :], AF.Copy, [b_PB[h], egt.b],
                                        [sball[tb].b], scale=egt.ap[:, c:c + 1])
                            else:
                                cpy(pexpB[:, hc], egt.ap[:, 7:8], [egt.b], [b_pexB])
                        defer(step)
                if main:
                    for half in range(2):
                        w = wload("Bog%d" % half, h)
                        for tb in range(NTB):
                            def ev(bank, tb=tb, half=half):
                                act_sigmoid(gate[tb][half].ap, bank.ap, Tl(t3.ap[:, 0:512], t3.b), [bank.b],
                                            [gate[tb][half].b])
                            group(w, tb, ev, 3)
                        w = wload("Bz%d" % half, h)
                        for tb in range(NTB):
                            def ev(bank, tb=tb, half=half):
                                act_sigmoid(t3.ap[:, 0:512], bank.ap, Tl(t3.ap[:, 0:512], t3.b), [bank.b], [t3.b])
                                tt(sz.ap, bank.ap, t3.ap[:, 0:512], ALU.mult, [bank.b, t3.b], [sz.b])
                                tt(gate[tb][half].ap, gate[tb][half].ap, sz.ap, ALU.mult,
                                   [sz.b, gate[tb][half].b], [gate[tb][half].b])
                            group(w, tb, ev, 3)
                    for tb in range(NTB):
                        def fB1(tb=tb, par=par):
                            specs = []
                            for c in range(8):
                                b4 = c // 2
                                cols = slice(c * 64, (c + 1) * 64)
                                q_c = qtB[par][tb].ap[:, cols]
                                s_c2 = scm[tb].ap[:, cols]
                                specs += [(OA0.ap[:, cols], sballv[tb][:, c, 0:128], q_c, True, False),
                                          (OA0.ap[:, cols], vptv[0][tb][:, b4, 0:128], s_c2, False, True),
                                          (OA1.ap[:, cols], sballv[tb][:, c, 128:256], q_c, True, False),
                                          (OA1.ap[:, cols], vptv[0][tb][:, b4, 128:256], s_c2, False, True),
                                          (MX.ap[0:1, cols], sballv[tb][:, c, 256:257], q_c, True, False),
                                          (MX.ap[0:1, cols], onesb[:, 0:1], s_c2, False, True)]
                            mm_group(specs, [sball[tb].b, qtB[par][tb].b, vpt[0][tb].b, scm[tb].b],
                                     [OA0.b, OA1.b, MX.b])
                            r0 = t0.ap[0:1, 0:512]
                            r1 = t1.ap[0:1, 0:512]
                            act(r0, MX.ap[0:1, :], AF.Abs, [MX.b], [t0.b])
                            ts(r0, r0, 1.0, ALU.max, [t0.b], [t0.b])
                            act(r0, r0, AF.Ln, [t0.b], [t0.b])
                            act(r0, r0, AF.Exp, [t0.b], [t0.b], scale=-1.0)
                            cpy(sq0.ap[0:1, :], r0, [t0.b], [sq0.b])
                            tt(r1, r0, sq0.ap[0:1, :], ALU.subtract, [t0.b, sq0.b], [t1.b])
                            cpy(sq1.ap[0:1, :], r1, [t1.b], [sq1.b])
                        defer(fB1)

                        def fB2(tb=tb):
                            mm_group([(MX.ap, onesb[0:1, :], sq0.ap[0:1, :], True, False),
                                      (MX.ap, onesb[0:1, :], sq1.ap[0:1, :], False, True)], [sq0.b, sq1.b], [MX.b])
                            a0, a1, a2 = t0.ap[:, 0:512], t1.ap[:, 0:512], t2.ap[:, 0:512]
                            act(a0, MX.ap, AF.Copy, [MX.b], [t0.b])
                            tt(a1, OA0.ap, a0, ALU.mult, [OA0.b, t0.b], [t1.b])
                            tt(a2, OA1.ap, a0, ALU.mult, [OA1.b, t0.b], [t2.b])
                            act(sq0.ap, a1, AF.Square, [t1.b], [sq0.b])
                            act(sq1.ap, a2, AF.Square, [t2.b], [sq1.b])

                        def fB3(tb=tb, h=h):
                            a1, a2, a3 = t1.ap[:, 0:512], t2.ap[:, 0:512], t3.ap[:, 0:512]
                            mm_group([(MX.ap, onesb[:, :], sq0.ap, True, False),
                                      (MX.ap, onesb[:, :], sq1.ap, False, True)], [sq0.b, sq1.b], [MX.b])
                            act_rpow(a3, MX.ap, [MX.b], [t3.b], 1.0 / 256, EPS, 0.5)
                            for half, a in ((0, a1), (1, a2)):
                                tb_ = (t1, t2)[half]
                                tt(a, a, a3, ALU.mult, [tb_.b, t3.b], [tb_.b])
                                ch = 16 + 2 * h + half
                                stt(oT[:, ch, tb * TB:(tb + 1) * TB], a, gnb[:, 2 * h + half:2 * h + half + 1],
                                    gate[tb][half].ap, ALU.mult, ALU.mult, [tb_.b, gate[tb][half].b], [])
                        defer(lambda f2=fB2, f3=fB3: (f2(), f3()))
            run_late()
            flush()
            pg.barrier()

        def heads_b_pre(main, last_pre):
            wk.reset()
            ebB = [wk.f32("ebB%d" % i, 512) for i in range(2)]
            wBb = [wk.f32("wBb%d" % i, 512) for i in range(2)]
            egB = [[wk.f32("egB%d%d" % (p_, i), 8) for i in range(2)] for p_ in range(2)]
            t0 = wk.f32("t0", 516)
            t1 = wk.f32("t1", 516)
            t2 = wk.f32("t2", 516)
            t3 = wk.f32("t3", 516)
            ktB = wk.bf("ktB", 512)
            vT = wk.bf("vT", 512)
            ktk = [wk.bf("ktk%d" % i, 512) for i in range(2)]
            vpt = [wk.bf("vpt%d" % i, 4 * 258) for i in range(2)]
            vptv = [v.ap.rearrange("p (b v) -> p b v", b=4) for v in vpt]
            ktkv = [k_.ap.rearrange("p (b v) -> p b v", b=4) for k_ in ktk]
            if main:
                qtB = [wk.bf("qtB%d" % i, 512) for i in range(2)]
                scm = [wk.bf("scmB%d" % i, 512) for i in range(2)]
                sball = [wk.bf("sballB%d" % i, 8 * 258) for i in range(2)]
                sballv = [s_.ap.rearrange("p (c v) -> p c v", c=8) for s_ in sball]
                gate = [[wk.bf("gB%d%d" % (i, j), 512) for j in range(2)] for i in range(2)]
                sz = wk.bf("szB", 512)
                sq0, sq1 = vT, sz
            for tb in range(NTB):
                pg.emit(DVE, lambda hh, a=vptv[tb][:, :, 256:258]: hh.memset(a, 1.0), [], [vpt[tb].b])
            ubanks = [UX, MX, SC, OA0] if main else [UX, MX, OA0, OA1]
            uc = [0]
            tpv = TP.ap.bitcast(BF16)

            def conv(bank, hidx, hist):
                cpy(t0.ap[:, 0:3], hist[:, hidx % 8, :], [b_hist], [t0.b])
                act(t0.ap[:, 3:515], bank.ap, AF.Copy, [bank.b], [t0.b])
                ts(t1.ap[:, 0:512], t0.ap[:, 0:512], cw[:, hidx, 0:1], ALU.mult, [t0.b], [t1.b],
                   s2=cbias[:, hidx:hidx + 1], op1=ALU.add)
                for k in range(1, 4):
                    stt(t1.ap[:, 0:512], t0.ap[:, k:k + 512], cw[:, hidx, k:k + 1], t1.ap[:, 0:512],
                        ALU.mult, ALU.add, [t0.b, t1.b], [t1.b])
                cpy(hist[:, hidx % 8, :], t0.ap[:, 512:515], [t0.b], [b_hist])

            for h in range(8):
                par = h % 2
                hc = slice(h, h + 1)
                nd0 = 1 if main else 4
                w1 = wload("Bfg", h)
                w2 = wload("Big", h)
                for tb in range(NTB):
                    def ev1(bank, tb=tb, par=par, hc=hc):
                        a0, a1 = t0.ap[:, 0:512], t1.ap[:, 0:512]
                        act(a0, bank.ap, AF.Exp, [bank.b], [t0.b], scale=-1.0, bias=nbfg[:, hc])
                        act(a0, a0, AF.Ln, [t0.b], [t0.b], bias=1.0)
                        pg.emit(DVE, lambda hh, o=a1, d1=a0: hh.tensor_tensor_scan(
                            out=o, data0=rmask, data1=d1, initial=0.0, op0=ALU.mult, op1=ALU.add),
                            [t0.b], [t1.b])
                        act(ebB[tb].ap, a1, AF.Exp, [t1.b], [ebB[tb].b], scale=-1.0)
                        act(wBb[tb].ap, a1, AF.Exp, [t1.b], [wBb[tb].b])
                        cpy(egB[par][tb].ap, ebB[tb].ap[:, 63:512:64], [ebB[tb].b], [egB[par][tb].b])
                    group(w1, tb, ev1, nd0)

                    def ev2(bank, tb=tb, hc=hc):
                        a0 = t0.ap[:, 0:512]
                        act(a0, bank.ap, AF.Exp, [bank.b], [t0.b], bias=big[:, hc])
                        tt(wBb[tb].ap, wBb[tb].ap, a0, ALU.mult, [t0.b, wBb[tb].b], [wBb[tb].b])
                    group(w2, tb, ev2, nd0)
                if main:
                    w = wload("Bq", h)
                    for tb in range(NTB):
                        def ev(bank, tb=tb, h=h):
                            conv(bank, h, histq)
                            act_sigmoid(t2.ap[:, 0:512], t1.ap[:, 0:512], Tl(t2.ap[:, 0:512], t2.b), [t1.b], [t2.b])
                            tt(t2.ap[:, 0:512], t2.ap[:, 0:512], t1.ap[:, 0:512], ALU.mult, [t1.b, t2.b], [t2.b])
                            stt(qtB[tb].ap, ebB[tb].ap, 128.0 ** -0.5, t2.ap[:, 0:512], ALU.mult, ALU.mult,
                                [ebB[tb].b, t2.b], [qtB[tb].b])
                        group(w, tb, ev, 1)
                elif last_pre:
                    w = wload("Bq", h)
                    bank = ipbank()
                    inproj(w, 1, bank)
                    cpy(histq[:, h, :], bank.ap[:, 509:512], [bank.b], [b_hist])
                run_late()
                flush()
                w = wload("Bk", h)
                for tb in range(NTB):
                    def ev(bank, tb=tb, h=h):
                        conv(bank, 8 + h, histk)
                        act_sigmoid(t2.ap[:, 0:512], t1.ap[:, 0:512], Tl(t2.ap[:, 0:512], t2.b), [t1.b], [t2.b])
                        tt(t2.ap[:, 0:512], t2.ap[:, 0:512], t1.ap[:, 0:512], ALU.mult, [t1.b, t2.b], [t2.b])
                        tt(ktB.ap, t2.ap[:, 0:512], wBb[tb].ap, ALU.mult, [t2.b, wBb[tb].b], [ktB.b])

                        def klate(tb=tb):
                            specs = [(tpv[:, b4 * 128:(b4 + 1) * 128], ktB.ap[:, b4 * 128:(b4 + 1) * 128], identB)
                                     for b4 in range(4)]
                            tr_group(specs, [ktB.b], [TP.b])
                            act(ktk[tb].ap, tpv[:, 0:512], AF.Copy, [TP.b], [ktk[tb].b])
                            if main:
                                specs = [(SC.ap[:, b4 * 128:(b4 + 1) * 128], ktB.ap[:, b4 * 128:(b4 + 1) * 128],
                                          qtB[tb].ap[:, b4 * 128:(b4 + 1) * 128], True, True) for b4 in range(4)]
                                mm_group(specs, [ktB.b, qtB[tb].b], [SC.b])
                                tt(scm[tb].ap, SC.ap, maskBD, ALU.mult, [SC.b], [scm[tb].b])
                        late.append(klate)
                    group(w, tb, ev, 0)
                for half, nm in ((0, "Bv0"), (1, "Bv1")):
                    w = wload(nm, h)
                    for tb in range(NTB):
                        def ev(bank, tb=tb, half=half):
                            act(vT.ap, bank.ap, AF.Copy, [bank.b], [vT.b])

                            def vlate(tb=tb, half=half):
                                specs = [(tpv[:, 512 + b4 * 128:512 + (b4 + 1) * 128],
                                          vT.ap[:, b4 * 128:(b4 + 1) * 128], identB) for b4 in range(4)]
                                tr_group(specs, [vT.b], [TP.b])
                                cpy(vptv[tb][:, :, half * 128:(half + 1) * 128],
                                    tpv[:, 512:1024].rearrange("p (b v) -> p b v", b=4), [TP.b], [vpt[tb].b])
                            late.append(vlate)
                        group(w, tb, ev, 0)
                for tb in range(NTB):
                    if main:
                        def sb0(tb=tb, h=h, hc=hc):
                            act(sballv[tb][:, 0, 0:257], PB[:, h, :], AF.Copy, [b_PB[h], b_pexB], [sball[tb].b],
                                scale=pexpB[:, hc])
                        defer(sb0)
                    for c in range(8):
                        def step(c=c, tb=tb, h=h, hc=hc, par=par):
                            b4 = c // 2
                            rows = slice((c % 2) * 64, (c % 2) * 64 + 64)
                            ub = ubanks[uc[0] % len(ubanks)]
                            uc[0] += 1
                            mm_group([(ub.ap[:, 0:257], ktkv[tb][rows, b4, :], vptv[tb][rows, b4, 0:257], True, True)],
                                     [ktk[tb].b, vpt[tb].b], [ub.b])
                            egt = egB[par][tb]
                            pe_ap = pexpB[:, hc] if c == 0 else egt.ap[:, c - 1:c]
                            stt(PB[:, h, :], PB[:, h, :], pe_ap, ub.ap[:, 0:257], ALU.mult, ALU.add,
                                [ub.b, egt.b, b_pexB], [b_PB[h]])
                            if c < 7:
                                if main:
                                    act(sballv[tb][:, c + 1, 0:257], PB[:, h, :], AF.Copy, [b_PB[h], egt.b],
                                        [sball[tb].b], scale=egt.ap[:, c:c + 1])
                            else:
                                cpy(pexpB[:, hc], egt.ap[:, 7:8], [egt.b], [b_pexB])
                        defer(step)
                if main:
                    for half in range(2):
                        w = wload("Bog%d" % half, h)
                        for tb in range(NTB):
                            def ev(bank, tb=tb, half=half):
                                act_sigmoid(gate[tb][half].ap, bank.ap, Tl(t3.ap[:, 0:512], t3.b), [bank.b], [gate[tb][half].b])
                            group(w, tb, ev, 3)
                        w = wload("Bz%d" % half, h)
                        for tb in range(NTB):
                            def ev(bank, tb=tb, half=half):
                                act_sigmoid(t3.ap[:, 0:512], bank.ap, Tl(t3.ap[:, 0:512], t3.b), [bank.b], [t3.b])
                                tt(sz.ap, bank.ap, t3.ap[:, 0:512], ALU.mult, [bank.b, t3.b], [sz.b])
                                tt(gate[tb][half].ap, gate[tb][half].ap, sz.ap, ALU.mult,
                                   [sz.b, gate[tb][half].b], [gate[tb][half].b])
                            group(w, tb, ev, 3)
                    for tb in range(NTB):
                        def fB1(tb=tb):
                            specs = []
                            for c in range(8):
                                b4 = c // 2
                                cols = slice(c * 64, (c + 1) * 64)
                                q_c = qtB[tb].ap[:, cols]
                                s_c2 = scm[tb].ap[:, cols]
                                specs += [(OA0.ap[:, cols], sballv[tb][:, c, 0:128], q_c, True, False),
                                          (OA0.ap[:, cols], vptv[tb][:, b4, 0:128], s_c2, False, True),
                                          (OA1.ap[:, cols], sballv[tb][:, c, 128:256], q_c, True, False),
                                          (OA1.ap[:, cols], vptv[tb][:, b4, 128:256], s_c2, False, True),
                                          (MX.ap[0:1, cols], sballv[tb][:, c, 256:257], q_c, True, False),
                                          (MX.ap[0:1, cols], onesb[:, 0:1], s_c2, False, True)]
                            mm_group(specs, [sball[tb].b, qtB[tb].b, vpt[tb].b, scm[tb].b], [OA0.b, OA1.b, MX.b])
                            r0 = t0.ap[0:1, 0:512]
                            r1 = t1.ap[0:1, 0:512]
                            act(r0, MX.ap[0:1, :], AF.Abs, [MX.b], [t0.b])
                            ts(r0, r0, 1.0, ALU.max, [t0.b], [t0.b])
                            act(r0, r0, AF.Ln, [t0.b], [t0.b])
                            act(r0, r0, AF.Exp, [t0.b], [t0.b], scale=-1.0)
                            cpy(sq0.ap[0:1, :], r0, [t0.b], [sq0.b])
                            tt(r1, r0, sq0.ap[0:1, :], ALU.subtract, [t0.b, sq0.b], [t1.b])
                            cpy(sq1.ap[0:1, :], r1, [t1.b], [sq1.b])
                        defer(fB1)

                        def fB2(tb=tb):
                            mm_group([(MX.ap, onesb[0:1, :], sq0.ap[0:1, :], True, False),
                                      (MX.ap, onesb[0:1, :], sq1.ap[0:1, :], False, True)], [sq0.b, sq1.b], [MX.b])
                            a0, a1, a2 = t0.ap[:, 0:512], t1.ap[:, 0:512], t2.ap[:, 0:512]
                            act(a0, MX.ap, AF.Copy, [MX.b], [t0.b])
                            tt(a1, OA0.ap, a0, ALU.mult, [OA0.b, t0.b], [t1.b])
                            tt(a2, OA1.ap, a0, ALU.mult, [OA1.b, t0.b], [t2.b])
                            act(sq0.ap, a1, AF.Square, [t1.b], [sq0.b])
                            act(sq1.ap, a2, AF.Square, [t2.b], [sq1.b])

                        def fB3(tb=tb, h=h):
                            a1, a2, a3 = t1.ap[:, 0:512], t2.ap[:, 0:512], t3.ap[:, 0:512]
                            mm_group([(MX.ap, onesb[:, :], sq0.ap, True, False),
                                      (MX.ap, onesb[:, :], sq1.ap, False, True)], [sq0.b, sq1.b], [MX.b])
                            act_rpow(a3, MX.ap, [MX.b], [t3.b], 1.0 / 256, EPS, 0.5)
                            for half, a in ((0, a1), (1, a2)):
                                tb_ = (t1, t2)[half]
                                tt(a, a, a3, ALU.mult, [tb_.b, t3.b], [tb_.b])
                                ch = 16 + 2 * h + half
                                stt(oT[:, ch, tb * TB:(tb + 1) * TB], a, gnb[:, 2 * h + half:2 * h + half + 1],
                                    gate[tb][half].ap, ALU.mult, ALU.mult, [tb_.b, gate[tb][half].b], [])
                        defer(lambda f2=fB2, f3=fB3: (f2(), f3()))
            run_late()
            flush()
            pg.barrier()

        def phase_b1():
            wk.reset()
            sga = [wk.f32("sga%d" % i, 512) for i in range(2)]
            sgb = [wk.f32("sgb%d" % i, 512) for i in range(2)]
            y1 = wk.f32("y1", 512)
            y2 = wk.f32("y2", 512)
            ybuf = [wk.bf("ybuf%d" % i, 1024) for i in range(2)]
            pool4 = [IP0, IP1, OA0, OA1]
            pc = 0
            b_ysc = pg.buf("ysc")
            for c in range(32):
                w = wload("Ga", c)
                for tb in range(NTB):
                    bank = pool4[pc % 4]; pc += 1
                    inproj(w, tb, bank)
                    act_sigmoid(sga[tb].ap, bank.ap, sga[tb], [bank.b], [sga[tb].b])
                w = wload("Gb", c)
                for tb in range(NTB):
                    bank = pool4[pc % 4]; pc += 1
                    inproj(w, tb, bank)
                    act_sigmoid(sgb[tb].ap, bank.ap, sgb[tb], [bank.b], [sgb[tb].b])
                w = wload("Up", c)
                yb = ybuf[c % 2]
                for tb in range(NTB):
                    ba = pool4[pc % 4]; pc += 1
                    bb_ = pool4[pc % 4]; pc += 1
                    inproj(w, tb, ba, 0, 16, src=oT)
                    inproj(w, tb, bb_, 16, 32, src=oT)
                    tt(y1.ap, ba.ap, sga[tb].ap, ALU.mult, [ba.b, sga[tb].b], [y1.b])
                    tt(y2.ap, bb_.ap, sgb[tb].ap, ALU.mult, [bb_.b, sgb[tb].b], [y2.b])
                    tt(yb.ap[:, tb * TB:(tb + 1) * TB], y1.ap, y2.ap, ALU.add, [y1.b, y2.b], [yb.b])
                dma(SP, s_ys[c % 2], ysc[c], yb.ap, [yb.b], [b_ysc])
            pg.barrier()

        def phase_b2(row0):
            yT = hT
            b_y = pg.buf("yT")
            for q4 in range(4):
                dma(SP, s_yl, yT[:, q4 * 8:(q4 + 1) * 8, :], ysc[q4 * 8:(q4 + 1) * 8].rearrange("c p t -> p c t"),
                    [], [b_y])
            ot = [Tl(R3[:, i * 1024:(i + 1) * 1024].bitcast(F32), pg.buf("ot%d" % i)) for i in range(4)]
            junk = Tl(R3[:, 4096:4608], pg.buf("junk"))
            b_sp = pg.buf("ssqp")
            b_osc = pg.buf("osc")
            pool4 = [IP0, IP1, OA0, OA1]
            pc = 0
            for cg in range(8):
                wsl = woslot[cg % 2]
                dma(POOL, s_wo[cg % 2], wsl.ap, wo[cg].rearrange("p (k j) -> p k j", k=KC), [], [wsl.b])
                for t8 in range(8):
                    bank = pool4[pc % 4]
                    o_t = ot[pc % 4]
                    s_o = s_ot[pc % 4]
                    pc += 1
                    specs = [(bank.ap, yT[:, k, t8 * P:(t8 + 1) * P], wsl.ap[:, k, :], k == 0, k == KC - 1)
                             for k in range(KC)]
                    mm_group(specs, [wsl.b, b_y], [bank.b])
                    act(o_t.ap, bank.ap, AF.Copy, [bank.b], [o_t.b])
                    act(junk.ap, bank.ap, AF.Square, [bank.b], [junk.b, b_sp], accum=ssqp[:, t8, cg:cg + 1])
                    dma(SP, s_o, osc[t8 * P:(t8 + 1) * P, cg * 512:(cg + 1) * 512], o_t.ap, [o_t.b], [b_osc])
            pg.barrier()
            gbt = Tl(R3[:, 20480:28672].bitcast(F32), pg.buf("gbt2"))
            dma(SP, s_gb, gbt.ap, gpost_d[0:1, :].partition_broadcast(P), [], [gbt.b])
            pg.emit(DVE, lambda h: h.reduce_sum(out=ssq, in_=ssqp, axis=mybir.AxisListType.X), [b_sp], [b_sp])
            act_rpow(rr, ssq, [b_sp], [b_sp], 1.0 / D, EPS, 0.5)
            ob = [Tl(R1[:, i * 8192:(i + 1) * 8192].bitcast(F32), pg.buf("ob%d" % i)) for i in range(2)]
            xb = [Tl(R1[:, (2 + i) * 8192:(3 + i) * 8192].bitcast(F32), pg.buf("xb%d" % i)) for i in range(2)]
            for t8 in range(8):
                s = t8 % 2
                dma(SP, s_ob[s], ob[s].ap, osc[t8 * P:(t8 + 1) * P, :], [], [ob[s].b])
                dma(SP, s_xb[s], xb[s].ap, xm[row0 + t8 * P:row0 + (t8 + 1) * P, :], [], [xb[s].b])
                stt(ob[s].ap, ob[s].ap, rr[:, t8:t8 + 1], gbt.ap, ALU.mult, ALU.mult, [ob[s].b, gbt.b, b_sp], [ob[s].b])
                tt(ob[s].ap, ob[s].ap, xb[s].ap, ALU.add, [ob[s].b, xb[s].b], [ob[s].b])
                dma(SP, s_st[s], y_d[row0 + t8 * P:row0 + (t8 + 1) * P, :], ob[s].ap, [ob[s].b], [])
            pg.barrier()

        class _Stop(Exception):
            pass

        def chk(name):
            if stop == name:
                pg.barrier()
                s_dbg = dsem("d_dbg")
                dma(SP, s_dbg, dbg_d[0], R1[:, :], [], [])
                dma(SP, s_dbg, dbg_d[1], R2[:, :], [], [])
                raise _Stop()

        try:
            chk("init")
            if not skip_pre:
                for seg in range(2):
                    phase_a0(xp, seg * T, gpre_d)
                    chk("pa0")
                    heads_a(False)
                    chk("pha")
                    heads_b_pre(False, seg == 1)
                    chk("phb")
            for h in range(16):
                ts(PA[:, h, :], PA[:, h, :], flag, ALU.mult, [b_PA[h], b_prm], [b_PA[h]])
            for h in range(8):
                ts(PB[:, h, :], PB[:, h, :], flag, ALU.mult, [b_PB[h], b_prm], [b_PB[h]])
            ts(sm[:, 72:120], sm[:, 72:120], flag, ALU.mult, [b_hist, b_prm], [b_hist])
            pg.barrier()
            for seg in range(2):
                phase_a0(xm, seg * T, gpre_d)
                chk("a0")
                heads_a(True)
                chk("ha")
                heads_b(True, False)
                chk("hb")
                phase_b1()
                chk("b1")
                phase_b2(seg * T)
                chk("b2")
        except _Stop:
            pass
        pg.final_wait(SP)

        with nc.Block() as block:
            @block.tensor
            def _(e):
                for f in PE.q:
                    f(e)

            @block.scalar
            def _(e):
                for f in ACT.q:
                    f(e)

            @block.vector
            def _(e):
                for f in DVE.q:
                    f(e)

            @block.gpsimd
            def _(e):
                for f in POOL.q:
                    f(e)

            @block.sync
            def _(e):
                for f in SP.q:
                    f(e)
        counts = {e.name: len(e.q) for e in pg.engs}
    return nc, counts


def _prep_weights(w_in, w_up_a, w_up_b, w_out):
    wt = np.empty((NT, P, KC * 128), np.float32)
    w_in = np.asarray(w_in, np.float32)
    for i, (nm, h) in enumerate(TILES):
        if nm == "Up":
            a = np.asarray(w_up_a[:, h * 128:(h + 1) * 128], np.float32).reshape(16, P, 128)
            b = np.asarray(w_up_b[:, h * 128:(h + 1) * 128], np.float32).reshape(16, P, 128)
            t = np.concatenate([a, b], axis=0)
        else:
            t = w_in[:, _tile_cols(nm, h)].reshape(KC, P, 128)
        wt[i] = t.transpose(1, 0, 2).reshape(P, KC * 128)
    wo = np.empty((8, P, KC * 512), np.float32)
    w_out = np.asarray(w_out, np.float32)
    for cg in range(8):
        t = w_out[:, cg * 512:(cg + 1) * 512].reshape(KC, P, 512)
        wo[cg] = t.transpose(1, 0, 2).reshape(P, KC * 512)
    return wt, wo


def _prep_consts():
    cst = np.zeros((P, 128 + 512 + 512), np.float32)
    cst[:, 0:128] = np.eye(P, dtype=np.float32)
    t = np.arange(512)
    cst[:, 128:640] = (t % 64 != 0).astype(np.float32)[None, :]
    s = np.arange(P)[:, None]
    tt_ = np.arange(P)[None, :]
    m = ((s // 64) == (tt_ // 64)) & (s <= tt_)
    cst[:, 640:1152] = np.tile(m.astype(np.float32), (1, 4))
    return cst


def _prep_params(lb_logits, conv_w, conv_b, b_ig, b_fg, g_norm_a, g_norm_b, half):
    prm = np.zeros((P, 168), np.float32)
    col = lambda v, n: np.asarray(v, np.float32).reshape(n, P).T
    prm[:, 0:16] = col(lb_logits[0], 16)
    prm[:, 16:32] = col(lb_logits[1], 16)
    prm[:, 32:48] = col(g_norm_a, 16)
    prm[:, 48:64] = col(g_norm_b, 16)
    cwv = np.asarray(conv_w, np.float32)
    prm[:, 64:128] = cwv.reshape(4, 16, P).transpose(2, 1, 0).reshape(P, 64)
    prm[:, 128:144] = col(conv_b, 16)
    prm[:, 144:152] = np.asarray(b_ig, np.float32).reshape(1, 8)
    prm[:, 152:160] = np.asarray(b_fg, np.float32).reshape(1, 8)
    prm[:, 160] = float(half)
    prm[0:8, 161] = np.asarray(b_fg, np.float32).reshape(8)
    prm[0:8, 162] = np.asarray(b_ig, np.float32).reshape(8)
    return prm


_CACHE = {}


def kernel(x, g_pre, w_in, lb_logits, conv_w, conv_b, b_ig, b_fg,
           g_norm_a, g_norm_b, w_up_a, w_up_b, w_out, g_post):
    x = np.asarray(x, np.float32)
    wt, wo = _prep_weights(np.asarray(w_in)[0], np.asarray(w_up_a)[0], np.asarray(w_up_b)[0],
                           np.asarray(w_out)[0])
    cst = _prep_consts()
    gpre = np.ascontiguousarray(np.asarray(g_pre, np.float32).reshape(1, D))
    gpost = np.ascontiguousarray(np.asarray(g_post, np.float32).reshape(1, D))
    if "nc" not in _CACHE:
        _CACHE["nc"] = build_program()
    nc, _ = _CACHE["nc"]
    in_maps = []
    for c in range(8):
        b, half = c // 2, c % 2
        prm = _prep_params(np.asarray(lb_logits), np.asarray(conv_w)[0], np.asarray(conv_b)[0],
                           np.asarray(b_ig)[0], np.asarray(b_fg)[0], np.asarray(g_norm_a)[0],
                           np.asarray(g_norm_b)[0], half)
        in_maps.append({
            "xm": np.ascontiguousarray(x[b, half * NTOK:(half + 1) * NTOK]),
            "xp": np.ascontiguousarray(x[b, 0:NTOK]),
            "wt": wt, "wo": wo, "prm": prm, "cst": cst, "gpre": gpre, "gpost": gpost,
        })
    res = run_bass_kernel_spmd(nc, in_maps, core_ids=list(range(8)))
    out = np.empty((4, 4096, D), np.float32)
    for c in range(8):
        b, half = c // 2, c % 2
        out[b, half * NTOK:(half + 1) * NTOK] = res.results[c]["y"]
    return out
```
